# Optimizing a Trainium2 kernel written in Bass

```python
import jax, jax.numpy as jnp
from jax import lax
import numpy as np


D_MODEL = 1024
BATCH = 8
SEQ = 4096
DEPTH = 4

MIX_WIDTH = D_MODEL
ATT_WIDTH = MIX_WIDTH // 2
RWKV_WIDTH = MIX_WIDTH - ATT_WIDTH
ATT_HEAD_DIM = 64
N_ATT_HEADS = ATT_WIDTH // ATT_HEAD_DIM
RWKV_HEAD_DIM = 64
N_RWKV_HEADS = RWKV_WIDTH // RWKV_HEAD_DIM
DILATED_PATTERNS = ((128, 1), (512, 4), (2048, 16))
ATT_BLOCK = 128
DECAY_LORA = 64
AAA_LORA = 64
VRES_LORA = 32
GATE_LORA = 128
RWKV_SHIFT_BASE = 3 * RWKV_WIDTH + DECAY_LORA + AAA_LORA + GATE_LORA
D_IN_BASE = 3 * ATT_WIDTH + RWKV_SHIFT_BASE
D_FF = 128 * ((8 * D_MODEL // 3 + 127) // 128)
N_EXPERTS = 8
TOP_K = 2
D_FF_EXPERT = 7 * D_MODEL // 2
N_DENSE = (DEPTH + 1) // 2
N_MOE = DEPTH // 2
DEEPNORM_ALPHA = float((2 * DEPTH) ** 0.25)
DEEPNORM_BETA = float((8 * DEPTH) ** -0.25)
LN_EPS = 1e-5
GN_EPS = 64e-5

kernel_name = 'hybrid_dilated_attn_rwkv7_moe_deepnorm'


def layer_norm(x, g, b):
    xf = x.astype(jnp.float32)
    mu = jnp.mean(xf, axis=-1, keepdims=True)
    var = jnp.mean(jnp.square(xf - mu), axis=-1, keepdims=True)
    return ((xf - mu) * lax.rsqrt(var + LN_EPS) * g + b).astype(x.dtype)


def token_shift(p, mu):
    prev = jnp.pad(p, ((0, 0), (1, 0), (0, 0)))[:, :-1]
    return p + (prev - p) * mu


def dilated_branch(q, k, v, window, dilation):
    B, S, H, Dh = q.shape
    steps = window // dilation
    L = S // dilation
    nb = -(-L // ATT_BLOCK)
    Lp = nb * ATT_BLOCK

    def blocks(t):
        t = t.reshape(B, L, dilation, H, Dh)
        t = jnp.pad(t, ((0, 0), (0, Lp - L), (0, 0), (0, 0), (0, 0)))
        return t.reshape(B, nb, ATT_BLOCK, dilation, H, Dh)

    def with_prev(t):
        prev = jnp.pad(t, ((0, 0), (1, 0), (0, 0), (0, 0), (0, 0), (0, 0)))[:, :-1]
        return jnp.concatenate([prev, t], axis=2)

    qb = blocks(q)
    kc = with_prev(blocks(k))
    vc = with_prev(blocks(v))
    s = jnp.einsum('bnqchd,bnkchd->bnchqk', qb, kc, preferred_element_type=jnp.float32)
    blk = jnp.arange(nb)[:, None] * ATT_BLOCK
    qi = blk + jnp.arange(ATT_BLOCK)[None, :]
    ki = blk + jnp.arange(2 * ATT_BLOCK)[None, :] - ATT_BLOCK
    dist = qi[:, :, None] - ki[:, None, :]
    valid = (dist >= 0) & (dist <= steps) & (ki[:, None, :] >= 0)
    s = jnp.where(valid[None, :, None, None], s, -jnp.inf)
    m = jnp.max(s, axis=-1, keepdims=True)
    p = jnp.exp(s - m)
    den = jnp.sum(p, axis=-1, keepdims=True)
    o = jnp.einsum('bnchqk,bnkchd->bnqchd', p / den, vc.astype(jnp.float32))
    lse = jnp.transpose((m + jnp.log(den))[..., 0], (0, 1, 4, 2, 3))
    o = o.reshape(B, Lp, dilation, H, Dh)[:, :L].reshape(B, S, H, Dh)
    lse = lse.reshape(B, Lp, dilation, H)[:, :L].reshape(B, S, H)
    return o, lse


def dilated_attention(q, k, v):
    outs, lses = [], []
    for window, dilation in DILATED_PATTERNS:
        o, lse = dilated_branch(q, k, v, window, dilation)
        outs.append(o)
        lses.append(lse)
    wts = jax.nn.softmax(jnp.stack(lses), axis=0)
    return jnp.sum(jnp.stack(outs) * wts[..., None], axis=0)


def rwkv7_time_mix(cols, v_first, decay_up, decay_base, aaa_up, aaa_base, gate_up,
                   k_k, k_a, r_k, lnx_g, lnx_b, vres_up, vres_base):
    B, S, _ = cols.shape
    H, N = N_RWKV_HEADS, RWKV_HEAD_DIM
    cuts = np.cumsum([RWKV_WIDTH] * 3 + [DECAY_LORA, AAA_LORA, GATE_LORA])
    r, k, v, wd, ad, gd, vd = jnp.split(cols, cuts, axis=-1)
    w_log = -jax.nn.softplus(-(decay_base + jnp.tanh(wd) @ decay_up)) - 0.5
    decay = jnp.exp(-jnp.exp(w_log))
    a = jax.nn.sigmoid(aaa_base + ad @ aaa_up)
    g = jax.nn.sigmoid(gd) @ gate_up
    if v_first is None:
        v_first = v
    else:
        v = v + (v_first - v) * jax.nn.sigmoid(vres_base + vd @ vres_up)
    heads = lambda t: t.reshape(B, S, H, N)
    kk = heads(k * k_k)
    kk = kk / jnp.maximum(jnp.sqrt(jnp.sum(kk * kk, axis=-1, keepdims=True)), 1e-12)
    k = k * (1.0 + (a - 1.0) * k_a)
    r_h, k_h, v_h, w_h, a_h = heads(r), heads(k), heads(v), heads(decay), heads(a)
    tm = lambda t: jnp.moveaxis(t, 1, 0)

    def step(state, inp):
        r_t, w_t, k_t, v_t, kk_t, a_t = inp
        sa = jnp.einsum('bhvk,bhk->bhv', state, -kk_t)
        state = (state * w_t[:, :, None, :] + sa[..., None] * (kk_t * a_t)[:, :, None, :]
                 + v_t[..., None] * k_t[:, :, None, :])
        return state, jnp.einsum('bhvk,bhk->bhv', state, r_t)

    state0 = jnp.zeros((B, H, N, N), jnp.float32)
    _, y = lax.scan(step, state0, (tm(r_h), tm(w_h), tm(k_h), tm(v_h), tm(kk), tm(a_h)))
    y = jnp.moveaxis(y, 0, 1)
    mu = jnp.mean(y, axis=-1, keepdims=True)
    var = jnp.mean(jnp.square(y - mu), axis=-1, keepdims=True)
    y = ((y - mu) * lax.rsqrt(var + GN_EPS)).reshape(B, S, RWKV_WIDTH) * lnx_g + lnx_b
    bonus = jnp.sum(r_h * k_h * r_k.reshape(H, N), axis=-1, keepdims=True) * v_h
    y = (y + bonus.reshape(B, S, RWKV_WIDTH)) * g
    return y, v_first


def hybrid_mixer(h, w_in_l, mu_l, w_out_l, v_first, decay_up, decay_base, aaa_up, aaa_base,
                 gate_up, k_k, k_a, r_k, lnx_g, lnx_b, vres_up, vres_base):
    B, S, _ = h.shape
    proj = h @ w_in_l
    q, k, v = jnp.split(proj[..., :3 * ATT_WIDTH], 3, axis=-1)
    ah = lambda t: t.reshape(B, S, N_ATT_HEADS, ATT_HEAD_DIM)
    att = dilated_attention(ah(q) * (ATT_HEAD_DIM ** -0.5), ah(k), ah(v)).reshape(B, S, ATT_WIDTH)
    cols = token_shift(proj[..., 3 * ATT_WIDTH:], mu_l).astype(jnp.float32)
    rw, v_first = rwkv7_time_mix(cols, v_first, decay_up, decay_base, aaa_up, aaa_base, gate_up,
                                 k_k, k_a, r_k, lnx_g, lnx_b, vres_up, vres_base)
    out = jnp.concatenate([att, rw], axis=-1).astype(h.dtype) @ w_out_l
    return out, v_first


def swiglu(h, wg, wu, wd):
    return (jax.nn.silu(h @ wg) * (h @ wu)) @ wd


def moe_swiglu(h, router_l, wg, wu, wd):
    logits = (h @ router_l).astype(jnp.float32)
    vals, idx = lax.top_k(logits, TOP_K)
    gates = jax.nn.softmax(vals, axis=-1)
    comb = jnp.sum(jax.nn.one_hot(idx, N_EXPERTS, dtype=jnp.float32) * gates[..., None], axis=-2)
    y = jnp.zeros(h.shape, jnp.float32)
    for e in range(N_EXPERTS):
        y = y + comb[..., e:e + 1] * swiglu(h, wg[e], wu[e], wd[e])
    return y.astype(h.dtype)


def setup_inputs(seed: int = 0) -> dict:
    key = jax.random.key(seed)
    ks = iter(jax.random.split(key, 40))
    nrm = lambda shape, scale: jax.random.normal(next(ks), shape, jnp.float32) * scale
    uni = lambda shape, lo, hi: jax.random.uniform(next(ks), shape, jnp.float32, lo, hi)
    L, D, W = DEPTH, D_MODEL, RWKV_WIDTH
    return {
        'x': nrm((BATCH, SEQ, D), 1.0),
        'w_in': nrm((L, D, D_IN_BASE), D ** -0.5),
        'w_in_vres': nrm((L - 1, D, VRES_LORA), D ** -0.5),
        'shift_mu': uni((L, RWKV_SHIFT_BASE), 0.0, 1.0),
        'shift_mu_vres': uni((L - 1, VRES_LORA), 0.0, 1.0),
        'decay_up': nrm((L, DECAY_LORA, W), 0.5 * DECAY_LORA ** -0.5),
        'decay_base': uni((L, W), -4.0, 1.0),
        'aaa_up': nrm((L, AAA_LORA, W), 0.5 * AAA_LORA ** -0.5),
        'aaa_base': nrm((L, W), 0.5),
        'vres_up': nrm((L - 1, VRES_LORA, W), 0.5 * VRES_LORA ** -0.5),
        'vres_base': nrm((L - 1, W), 0.5),
        'gate_up': nrm((L, GATE_LORA, W), GATE_LORA ** -0.5),
        'k_k': 0.85 + nrm((L, W), 0.05),
        'k_a': 1.0 + nrm((L, W), 0.05),
        'r_k': nrm((L, W), 0.1),
        'lnx_g': 1.0 + nrm((L, W), 0.05),
        'lnx_b': nrm((L, W), 0.02),
        'w_out': nrm((L, MIX_WIDTH, D), DEEPNORM_BETA * MIX_WIDTH ** -0.5),
        'ln1_g': 1.0 + nrm((L, D), 0.05),
        'ln1_b': nrm((L, D), 0.02),
        'ln2_g': 1.0 + nrm((L, D), 0.05),
        'ln2_b': nrm((L, D), 0.02),
        'ffn_w_gate': nrm((N_DENSE, D, D_FF), D ** -0.5),
        'ffn_w_up': nrm((N_DENSE, D, D_FF), D ** -0.5),
        'ffn_w_down': nrm((N_DENSE, D_FF, D), DEEPNORM_BETA * D_FF ** -0.5),
        'router': nrm((N_MOE, D, N_EXPERTS), D ** -0.5),
        'moe_w_gate': nrm((N_MOE, N_EXPERTS, D, D_FF_EXPERT), D ** -0.5),
        'moe_w_up': nrm((N_MOE, N_EXPERTS, D, D_FF_EXPERT), D ** -0.5),
        'moe_w_down': nrm((N_MOE, N_EXPERTS, D_FF_EXPERT, D), DEEPNORM_BETA * D_FF_EXPERT ** -0.5),
    }


def reference(x, w_in, w_in_vres, shift_mu, shift_mu_vres, decay_up, decay_base, aaa_up,
              aaa_base, vres_up, vres_base, gate_up, k_k, k_a, r_k, lnx_g, lnx_b, w_out,
              ln1_g, ln1_b, ln2_g, ln2_b, ffn_w_gate, ffn_w_up, ffn_w_down, router,
              moe_w_gate, moe_w_up, moe_w_down):
    v_first = None
    for l in range(DEPTH):
        if l == 0:
            w_in_l, mu_l, vu, vb = w_in[0], shift_mu[0], None, None
        else:
            w_in_l = jnp.concatenate([w_in[l], w_in_vres[l - 1]], axis=1)
            mu_l = jnp.concatenate([shift_mu[l], shift_mu_vres[l - 1]], axis=0)
            vu, vb = vres_up[l - 1], vres_base[l - 1]
        mix, v_first = hybrid_mixer(x, w_in_l, mu_l, w_out[l], v_first, decay_up[l], decay_base[l],
                                    aaa_up[l], aaa_base[l], gate_up[l], k_k[l], k_a[l], r_k[l],
                                    lnx_g[l], lnx_b[l], vu, vb)
        x = layer_norm(DEEPNORM_ALPHA * x + mix, ln1_g[l], ln1_b[l])
        if l % 2 == 0:
            i = l // 2
            f = swiglu(x, ffn_w_gate[i], ffn_w_up[i], ffn_w_down[i])
        else:
            i = l // 2
            f = moe_swiglu(x, router[i], moe_w_gate[i], moe_w_up[i], moe_w_down[i])
        x = layer_norm(DEEPNORM_ALPHA * x + f, ln2_g[l], ln2_b[l])
    return x
```

```python
import numpy as np
import concourse.bass as bass
import concourse.mybir as mybir
from concourse.bass_utils import run_bass_kernel_spmd
from contextlib import ExitStack

F32 = mybir.dt.float32
BF16 = mybir.dt.bfloat16
AF = mybir.ActivationFunctionType
ALU = mybir.AluOpType
AX = mybir.AxisListType

S = 4096
D = 1024
DEPTH = 4
NT = S // 128
ALPHA = float((2 * DEPTH) ** 0.25)
LN_EPS = 1e-5
GN_EPS = 64e-5
CDEC = float(np.exp(-0.5))
NEG = -30000.0
DFF = 2816
DFE = 3584
NE = 8

import os
RMODE = int(os.environ.get('RMODE', '3'))
SEM_CH = 12000
N_ESEM = 12
N_DSEM = 24


class T:
    __slots__ = ('h', 'name', 'lastw', 'rd_eng', 'rd_dma')

    ALL = []

    def __init__(self, h, name=''):
        self.h = h
        self.name = name
        self.lastw = None
        self.rd_eng = {}
        self.rd_dma = []
        T.ALL.append(self)

    def __getitem__(self, k):
        return self.h[k]


class Op:
    __slots__ = ('eng', 'fn', 'deps', 'is_dma', 'marked', 'done', 'slot_prev')

    def __init__(self, eng, fn, is_dma):
        self.eng = eng
        self.fn = fn
        self.deps = []
        self.is_dma = is_dma
        self.marked = is_dma
        self.done = None
        self.slot_prev = None


class Prog:
    ENGS = ['pe', 'act', 'dve', 'pool', 'sp']

    def __init__(self, nc, es):
        T.ALL.clear()
        self.nc = nc
        self.es = es
        self.ops = {e: [] for e in self.ENGS}
        self.esem = {e: [es.enter_context(nc.semaphore(f's_{e}_{i}')) for i in range(N_ESEM)]
                     for e in self.ENGS}
        self.dsem = [es.enter_context(nc.semaphore(f'd_{i}')) for i in range(N_DSEM)]
        self.dcount = [0] * N_DSEM
        self.dnext = 0
        self.dma_since_barrier = []
        self.n_tiles = 0

    def sb(self, shape, dtype, name=None):
        self.n_tiles += 1
        name = name or 't'
        h = self.es.enter_context(self.nc.sbuf_tensor(f'{name}_{self.n_tiles}', list(shape), dtype))
        return T(h, name)

    def ps(self, shape, dtype=F32, name=None):
        self.n_tiles += 1
        name = name or 'p'
        h = self.es.enter_context(self.nc.psum_tensor(f'{name}_{self.n_tiles}', list(shape), dtype))
        return T(h, name)

    def add(self, eng, fn, r=(), w=(), dma=False):
        op = Op(eng, fn, dma)
        deps = []
        for t in r:
            if t is None:
                continue
            if t.lastw is not None:
                deps.append(t.lastw)
        for t in w:
            if t is None:
                continue
            if t.lastw is not None:
                deps.append(t.lastw)
            deps.extend(t.rd_eng.values())
            deps.extend(t.rd_dma)
        seen = set()
        for d in deps:
            if d is op or id(d) in seen:
                continue
            seen.add(id(d))
            if (not d.is_dma) and (not dma) and d.eng == eng and eng == 'pe':
                continue
            d.marked = True
            op.deps.append(d)
        for t in r:
            if t is None:
                continue
            if dma:
                t.rd_dma.append(op)
            else:
                t.rd_eng[eng] = op
        for t in w:
            if t is None:
                continue
            t.lastw = op
            t.rd_eng = {}
            t.rd_dma = []
        if dma:
            s = self.dnext
            self.dnext = (self.dnext + 1) % N_DSEM
            op.slot_prev = (s, self.dcount[s])
            self.dcount[s] += 16
            op.done = (self.dsem[s], self.dcount[s])
            self.dma_since_barrier.append(op)
        self.ops[eng].append(op)
        return op

    def barrier(self):
        lasts = []
        for e in self.ENGS:
            for o in reversed(self.ops[e]):
                if not o.is_dma:
                    lasts.append(o)
                    break
        dmas = list(self.dma_since_barrier)
        self.dma_since_barrier = []
        for e in self.ENGS:
            op = Op(e, lambda eng: eng.nop(), False)
            for d in lasts + dmas:
                d.marked = True
                op.deps.append(d)
            self.ops[e].append(op)
        for t in T.ALL:
            t.lastw = None
            t.rd_eng = {}
            t.rd_dma = []

    def emit(self):
        nc = self.nc
        for e in self.ENGS:
            n = 0
            for o in self.ops[e]:
                if o.is_dma:
                    continue
                if o.marked:
                    n += 1
                    si = (n - 1) // SEM_CH
                    assert si < N_ESEM, f'too many marked ops on {e}: {n}'
                    o.done = (self.esem[e][si], (n - 1) % SEM_CH + 1)
        engmap = {'pe': 'tensor', 'act': 'scalar', 'dve': 'vector', 'pool': 'gpsimd', 'sp': 'sync'}
        with nc.Block() as block:
            for e in self.ENGS:
                ops = self.ops[e]

                def body(eng, ops=ops):
                    waited = {}
                    for o in ops:
                        ws = [d.done for d in o.deps]
                        if o.is_dma and o.slot_prev[1] > 0:
                            ws.append((self.dsem[o.slot_prev[0]], o.slot_prev[1]))
                        for (sem, val) in ws:
                            k = id(sem)
                            if waited.get(k, 0) >= val:
                                continue
                            waited[k] = val
                            eng.wait_ge(sem, val)
                        inst = o.fn(eng)
                        if o.is_dma:
                            inst.then_inc(o.done[0], 16)
                        elif o.marked:
                            inst.then_inc(o.done[0], 1)

                getattr(block, engmap[e])(body)
        return {e: len(self.ops[e]) for e in self.ENGS}

    @staticmethod
    def _l(x):
        if x is None:
            return []
        return list(x) if isinstance(x, (list, tuple)) else [x]

    def mm(self, wT, o, l, r, rd, start=True, stop=True):
        return self.add('pe', lambda e: e.matmul(o, lhsT=l, rhs=r, start=start, stop=stop), r=self._l(rd), w=self._l(wT))

    def tr(self, wT, o, i, idn, rd):
        return self.add('pe', lambda e: e.transpose(o, i, idn), r=self._l(rd), w=self._l(wT))

    def act(self, wT, o, i, func, rd, scale=None, bias=None):
        kw = {}
        if scale is not None:
            kw['scale'] = scale
        if bias is not None:
            kw['bias'] = bias
        return self.add('act', lambda e: e.activation(out=o, in_=i, func=func, **kw), r=self._l(rd), w=self._l(wT))

    def tt(self, eng, wT, o, a, b, op, rd):
        return self.add(eng, lambda e: e.tensor_tensor(out=o, in0=a, in1=b, op=op), r=self._l(rd), w=self._l(wT))

    def ts(self, eng, wT, o, a, s1, s2, op0, op1, rd):
        if op1 is None:
            return self.add(eng, lambda e: e.tensor_scalar(out=o, in0=a, scalar1=s1, scalar2=None, op0=op0),
                            r=self._l(rd), w=self._l(wT))
        return self.add(eng, lambda e: e.tensor_scalar(out=o, in0=a, scalar1=s1, scalar2=s2, op0=op0, op1=op1),
                        r=self._l(rd), w=self._l(wT))

    def stt(self, wT, o, a, sc, b, op0, op1, rd):
        return self.add('dve', lambda e: e.scalar_tensor_tensor(out=o, in0=a, scalar=sc, in1=b, op0=op0, op1=op1),
                        r=self._l(rd), w=self._l(wT))

    def red(self, wT, o, i, rd, op=None):
        op = op or ALU.add
        return self.add('dve', lambda e: e.tensor_reduce(out=o, in_=i, axis=AX.X, op=op), r=self._l(rd), w=self._l(wT))

    def rcp(self, wT, o, i, rd):
        return self.add('dve', lambda e: e.reciprocal(out=o, in_=i), r=self._l(rd), w=self._l(wT))

    def cp(self, eng, wT, o, i, rd):
        if eng == 'act':
            return self.add('act', lambda e: e.activation(out=o, in_=i, func=AF.Copy), r=self._l(rd), w=self._l(wT))
        return self.add(eng, lambda e: e.tensor_copy(out=o, in_=i), r=self._l(rd), w=self._l(wT))

    def ms(self, eng, wT, o, val):
        return self.add(eng, lambda e: e.memset(o, val), w=self._l(wT))

    def dma(self, eng, wT, o, i, rd=None):
        return self.add(eng, lambda e: e.dma_start(out=o, in_=i), r=self._l(rd), w=self._l(wT), dma=True)

    def asel(self, wT, o, i, pattern, cmp, fill, base, cm, rd):
        return self.add('pool', lambda e: e.affine_select(out=o, in_=i, pattern=pattern, compare_op=cmp, fill=fill,
                                                          base=base, channel_multiplier=cm), r=self._l(rd), w=self._l(wT))


def build_consts(p):
    C = {}
    ones = p.sb([128, 128], F32, 'ones')
    zeros = p.sb([128, 128], F32, 'zeros')
    p.ms('pool', ones, ones[:], 1.0)
    p.ms('pool', zeros, zeros[:], 0.0)
    idf = p.sb([128, 128], F32, 'idf')
    p.asel(idf, idf[:], ones[:], [[-1, 128]], ALU.is_equal, 0.0, 0, 1, ones)
    idb = p.sb([128, 128], BF16, 'idb')
    p.cp('pool', idb, idb[:], idf[:], idf)
    mbo32 = p.sb([128, 128], F32, 'mbo32')
    mbp32 = p.sb([128, 128], F32, 'mbp32')
    p.asel(mbo32, mbo32[:], zeros[:], [[1, 128]], ALU.is_ge, NEG, 0, -1, zeros)
    p.asel(mbp32, mbp32[:], zeros[:], [[-1, 128]], ALU.is_ge, NEG, 0, 1, zeros)
    mb = p.sb([128, 256], BF16, 'mb')
    p.cp('pool', mb, mb[:, 0:128], mbp32[:], mbp32)
    p.cp('pool', mb, mb[:, 128:256], mbo32[:], mbo32)
    mL = p.sb([64, 64], F32, 'mL')
    mU = p.sb([64, 64], F32, 'mU')
    mUI = p.sb([64, 64], F32, 'mUI')
    p.asel(mL, mL[:], ones[0:64, 0:64], [[-1, 64]], ALU.is_gt, 0.0, 0, 1, ones)
    p.asel(mU, mU[:], ones[0:64, 0:64], [[1, 64]], ALU.is_gt, 0.0, 0, -1, ones)
    p.asel(mUI, mUI[:], ones[0:64, 0:64], [[1, 64]], ALU.is_ge, 0.0, 0, -1, ones)
    tri = p.sb([128, 128], F32, 'tri')
    p.asel(tri, tri[:], ones[:], [[1, 128]], ALU.is_ge, 0.0, 0, -1, ones)
    p.ms('pool', tri, tri[0:64, 64:128], 0.0)
    tot = p.sb([128, 128], F32, 'tot')
    p.ms('pool', tot, tot[:], 0.0)
    p.ms('pool', tot, tot[0:64, 0:64], 1.0)
    p.ms('pool', tot, tot[64:128, 64:128], 1.0)
    cind = p.sb([128, 2], F32, 'cind')
    p.ms('pool', cind, cind[:], 0.0)
    p.ms('pool', cind, cind[0:64, 0:1], 1.0)
    p.ms('pool', cind, cind[64:128, 1:2], 1.0)
    onesb = p.sb([128, 64], BF16, 'onesb')
    p.ms('pool', onesb, onesb[:], 1.0)
    C.update(ones=ones, zeros=zeros, idf=idf, idb=idb, mb=mb, mL=mL, mU=mU, mUI=mUI, tri=tri, tot=tot,
             cind=cind, onesb=onesb)
    return C


def phase1a(p, C, G, l, xin):
    hT, hTt, banks = G['hT'], G['hTt'], G['banks']
    W = G['W']
    with ExitStack() as es:
        p.es = es
        wqk = p.sb([128, 8, 1024], BF16, 'wqk')
        p.dma('pool', wqk, wqk[:], W['w_in'][l, :, 0:1024].rearrange("(c p) f -> p c f", p=128))
        xt = [p.sb([128, 1024], F32, 'xt') for _ in range(2)]
        qks = [p.sb([128, 8, 512], BF16, 'qks') for _ in range(2)]
        for i in range(NT):
            x_ = xt[i % 2]
            p.dma('sp', x_, x_[:], xin[i * 128:(i + 1) * 128, :])
            for hb in range(2):
                bk = banks[hb]
                for cc in range(4):
                    c = hb * 4 + cc
                    p.tr(bk, bk[:, cc * 128:(cc + 1) * 128], x_[:, c * 128:(c + 1) * 128], C['idf'][:], [x_, C['idf']])
                eng = 'act' if hb == 0 else 'dve'
                p.cp(eng, hTt[i], hT[:, hb * 4:(hb + 1) * 4, 2 + i * 128:2 + (i + 1) * 128],
                     bk[:, :].rearrange("p (c t) -> p c t", c=4), bk)
            if i % 4 == 3:
                g = i // 4
                st = qks[g % 2]
                rd = [hTt[j] for j in range(g * 4, g * 4 + 4)]
                for m in range(8):
                    bk = banks[2 + (m % 2)]
                    for c in range(8):
                        p.mm(bk, bk[:, :], wqk[:, c, m * 128:(m + 1) * 128],
                             hT[:, c, 2 + g * 512:2 + (g + 1) * 512], [wqk] + rd, start=(c == 0), stop=(c == 7))
                    sc = 0.125 if m < 4 else 1.0
                    p.act(st, st[:, m, :], bk[:, :], AF.Identity, bk, scale=sc)
                p.dma('sp', None, G['qk_d'][:, :, g * 512:(g + 1) * 512].rearrange("m p t -> p m t"), st[:], st)
    p.es = G['es']
    p.barrier()


def phase1b(p, C, G, l):
    hT, hTt, banks = G['hT'], G['hTt'], G['banks']
    W = G['W']
    NW = 1792 + (32 if l > 0 else 0)
    b0, b1, b2, b3, b4, b5, b6, b7 = banks
    with ExitStack() as es:
        p.es = es
        wa = p.sb([128, 8, 1824], BF16, 'wa')
        wb = p.sb([128, 8, 1824], BF16, 'wb')
        with ExitStack() as es2:
            p.es = es2
            mu = p.sb([128, 1824], F32, 'mu')
            omu = p.sb([128, 1824], F32, 'omu')
            p.dma('sp', mu, mu[:, 0:1792], W['shift_mu'][l:l + 1, :].partition_broadcast(128))
            if l > 0:
                p.dma('sp', mu, mu[:, 1792:1824], W['shift_mu_vres'][l - 1:l, :].partition_broadcast(128))
            p.ts('dve', omu, omu[:, 0:NW], mu[:, 0:NW], -1.0, 1.0, ALU.mult, ALU.add, mu)
            stg = [p.sb([128, 1824], F32, 'stg') for _ in range(2)]
            for c in range(8):
                s_ = stg[c % 2]
                p.dma('sp', s_, s_[:, 0:1792], W['w_in'][l, c * 128:(c + 1) * 128, 1536:3328])
                if l > 0:
                    p.dma('sp', s_, s_[:, 1792:1824], W['w_in_vres'][l - 1, c * 128:(c + 1) * 128, :])
                p.tt('dve', wa, wa[:, c, 0:NW], s_[:, 0:NW], omu[:, 0:NW], ALU.mult, [s_, omu])
                p.tt('pool', wb, wb[:, c, 0:NW], s_[:, 0:NW], mu[:, 0:NW], ALU.mult, [s_, mu])
            p.barrier()
        p.es = es
        names = ['decay_base', 'aaa_base', 'k_k', 'k_a', 'r_k']
        bc = {}
        for n in names:
            bc[n] = p.sb([128, 512], F32, 'bc_' + n)
            p.dma('sp', bc[n], bc[n][:], W[n][l:l + 1, :].partition_broadcast(128))
        if l > 0:
            bc['vres_base'] = p.sb([128, 512], F32, 'bc_vb')
            p.dma('sp', bc['vres_base'], bc['vres_base'][:], W['vres_base'][l - 1:l, :].partition_broadcast(128))
        lup1 = p.sb([128, 512], BF16, 'lup1')
        p.dma('pool', lup1, lup1[0:64, :], W['decay_up'][l])
        p.dma('pool', lup1, lup1[64:128, :], W['aaa_up'][l])
        gup = p.sb([128, 512], BF16, 'gup')
        p.dma('pool', gup, gup[:], W['gate_up'][l])
        if l > 0:
            vup = p.sb([32, 512], BF16, 'vup')
            p.dma('pool', vup, vup[:], W['vres_up'][l - 1])
        f = lambda n: p.sb([128, 512], F32, n)
        T1, T2, SW, AA, GG, VV, VF, KK, KP, BB, CS = [f(n) for n in
                                                      ['T1', 'T2', 'SW', 'AA', 'GG', 'VV', 'VF', 'KK', 'KP', 'BB', 'CS']]
        TMS = p.sb([128, 4, 512], BF16, 'TMS')
        RT = p.sb([128, 512], BF16, 'RT')
        BT = p.sb([128, 512], BF16, 'BT')
        KT = p.sb([128, 512], BF16, 'KT')
        FTS = p.sb([64, 2, 2048], BF16, 'FTS')
        GC = p.sb([64, 2, 8], F32, 'GC')
        sm = lambda n, w_: p.sb([128, w_], F32, n)
        ssq, rn, rkb = sm('ssq', 8), sm('rn', 8), sm('rkb', 8)
        lo1 = p.sb([128, 512], BF16, 'lo1')
        lo2 = p.sb([128, 512], BF16, 'lo2')
        lo3 = p.sb([32, 512], BF16, 'lo3')
        h8 = lambda ap: ap.rearrange("p (h k) -> p h k", h=8)

        for g in range(NT // 4):
            rd = [hTt[j] for j in range(max(0, g * 4 - 1), g * 4 + 4)]
            specs = [(1536, 128, lo1, b0), (1664, 128, lo2, b1)]
            if l > 0:
                specs.append((1792, 32, lo3, b0))
            for (c0, wd_, lo, bk) in specs:
                for c in range(8):
                    p.mm(bk, bk[0:wd_, :], wa[:, c, c0:c0 + wd_], hT[:, c, 2 + g * 512:2 + (g + 1) * 512],
                         [wa] + rd, start=(c == 0), stop=False)
                    p.mm(bk, bk[0:wd_, :], wb[:, c, c0:c0 + wd_], hT[:, c, 1 + g * 512:1 + (g + 1) * 512],
                         [wb] + rd, start=False, stop=(c == 7))
                if lo is lo1:
                    p.act(lo, lo[0:64, :], bk[0:64, :], AF.Tanh, bk)
                    p.cp('dve', lo, lo[64:128, :], bk[64:128, :], bk)
                elif lo is lo2:
                    p.act(lo, lo[:, :], bk[:, :], AF.Sigmoid, bk)
                else:
                    p.cp('dve', lo, lo[0:32, :], bk[0:32, :], bk)
            for ti in range(4):
                i = g * 4 + ti
                t0 = i * 128
                ts_ = slice(ti * 128, (ti + 1) * 128)
                rdh = [hTt[j] for j in range(max(0, i - 1), i + 1)]
                if l > 0:
                    p.dma('sp', VF, VF[:], G['vfirst_d'][t0:t0 + 128, :])
                for n, bk in enumerate([b2, b3, b4]):
                    for c in range(8):
                        p.mm(bk, bk[:, :], hT[:, c, 2 + t0:2 + t0 + 128], wa[:, c, n * 512:(n + 1) * 512],
                             [wa] + rdh, start=(c == 0), stop=False)
                        p.mm(bk, bk[:, :], hT[:, c, 1 + t0:1 + t0 + 128], wb[:, c, n * 512:(n + 1) * 512],
                             [wb] + rdh, start=False, stop=(c == 7))
                p.mm(b5, b5[:, :], lo1[0:64, ts_], lup1[0:64, :], [lo1, lup1])
                p.mm(b6, b6[:, :], lo1[64:128, ts_], lup1[64:128, :], [lo1, lup1])
                p.mm(b7, b7[:, :], lo2[:, ts_], gup[:, :], [lo2, gup])
                p.tt('dve', T1, T1[:], b5[:, :], bc['decay_base'][:], ALU.add, [b5, bc['decay_base']])
                p.act(SW, SW[:], T1[:], AF.Sigmoid, T1)
                p.tt('dve', T2, T2[:], b6[:, :], bc['aaa_base'][:], ALU.add, [b6, bc['aaa_base']])
                p.act(AA, AA[:], T2[:], AF.Sigmoid, T2)
                p.cp('act', GG, GG[:], b7[:, :], b7)
                p.dma('sp', None, G['g_d'][t0:t0 + 128, :], GG[:], GG)
                if l == 0:
                    p.cp('act', VV, VV[:], b4[:, :], b4)
                    p.dma('sp', None, G['vfirst_d'][t0:t0 + 128, :], VV[:], VV)
                else:
                    p.mm(b0, b0[:, :], lo3[0:32, ts_], vup[0:32, :], [lo3, vup])
                    p.tt('dve', T2, T2[:], b0[:, :], bc['vres_base'][:], ALU.add, [b0, bc['vres_base']])
                    p.act(T2, T2[:], T2[:], AF.Sigmoid, T2)
                    p.tt('dve', VV, VV[:], VF[:], b4[:, :], ALU.subtract, [VF, b4])
                    p.tt('dve', VV, VV[:], VV[:], T2[:], ALU.mult, [VV, T2])
                    p.tt('dve', VV, VV[:], VV[:], b4[:, :], ALU.add, [VV, b4])
                p.dma('sp', None, G['v_d'][t0:t0 + 128, :], VV[:], VV)
                p.cp('act', TMS, TMS[:, 3, :], VV[:], VV)
                p.tt('dve', KK, KK[:], b3[:, :], bc['k_k'][:], ALU.mult, [b3, bc['k_k']])
                p.tt('dve', T1, T1[:], KK[:], KK[:], ALU.mult, [KK])
                p.red(ssq, ssq[:], h8(T1[:]), T1)
                p.act(rn, rn[:], ssq[:], AF.Sqrt, ssq)
                p.ts('dve', rn, rn[:], rn[:], 1e-12, None, ALU.max, None, rn)
                p.rcp(rn, rn[:], rn[:], rn)
                p.tt('dve', KK, h8(KK[:]), h8(KK[:]), rn[:].unsqueeze(2).to_broadcast([128, 8, 64]), ALU.mult, [KK, rn])
                p.stt(T1, T1[:], AA[:], -1.0, bc['k_a'][:], ALU.add, ALU.mult, [AA, bc['k_a']])
                p.stt(KP, KP[:], T1[:], 1.0, b3[:, :], ALU.add, ALU.mult, [T1, b3])
                p.tt('dve', BB, BB[:], KK[:], AA[:], ALU.mult, [KK, AA])
                p.tt('dve', T1, T1[:], b2[:, :], bc['r_k'][:], ALU.mult, [b2, bc['r_k']])
                p.tt('dve', T1, T1[:], T1[:], KP[:], ALU.mult, [T1, KP])
                p.red(rkb, rkb[:], h8(T1[:]), T1)
                p.dma('sp', None, G['rk_d'][t0:t0 + 128, :], rkb[:], rkb)
                p.mm(b5, b5[:, :], C['tri'][:], SW[:], [C['tri'], SW])
                p.mm(b6, b6[:, :], C['tot'][:], SW[:], [C['tot'], SW])
                gcv = b7[0:64, 0:16].rearrange("p (c h) -> p c h", c=2)
                for h in range(8):
                    p.mm(b7, gcv[:, :, h], SW[:, h * 64:(h + 1) * 64], C['cind'][:], [SW, C['cind']])
                p.act(GC, GC[:], gcv, AF.Exp, b7, scale=-CDEC)
                p.dma('sp', None, G['gc_d'][2 * i:2 * i + 2].rearrange("c k h -> k c h"), GC[:], GC)
                p.cp('act', CS, CS[:], b5[:, :], b5)
                p.act(T1, T1[:], CS[:], AF.Exp, CS, scale=-CDEC)
                p.tt('dve', RT, RT[:], b2[:, :], T1[:], ALU.mult, [b2, T1])
                p.tt('dve', T1, T1[:], CS[:], SW[:], ALU.subtract, [CS, SW])
                p.act(T1, T1[:], T1[:], AF.Exp, T1, scale=-CDEC)
                p.stt(TMS, TMS[:, 0, :], KK[:], -1.0, T1[:], ALU.mult, ALU.mult, [KK, T1])
                p.act(T1, T1[:], CS[:], AF.Exp, CS, scale=CDEC)
                p.tt('dve', BT, BT[:], BB[:], T1[:], ALU.mult, [BB, T1])
                p.tt('dve', KT, KT[:], KP[:], T1[:], ALU.mult, [KP, T1])
                p.tt('dve', T2, T2[:], b6[:, :], CS[:], ALU.subtract, [b6, CS])
                p.act(T2, T2[:], T2[:], AF.Exp, T2, scale=-CDEC)
                p.tt('dve', TMS, TMS[:, 1, :], BB[:], T2[:], ALU.mult, [BB, T2])
                p.tt('dve', TMS, TMS[:, 2, :], KP[:], T2[:], ALU.mult, [KP, T2])
                p.dma('sp', None, G['tm_d'][2 * i:2 * i + 2].rearrange("c s t f -> (c s) t f"), TMS[:], TMS)
                srcs = [(TMS, lambda r_, h_: TMS[r_, 0, h_ * 64:(h_ + 1) * 64]),
                        (RT, lambda r_, h_: RT[r_, h_ * 64:(h_ + 1) * 64]),
                        (BT, lambda r_, h_: BT[r_, h_ * 64:(h_ + 1) * 64]),
                        (KT, lambda r_, h_: KT[r_, h_ * 64:(h_ + 1) * 64])]
                for ck in range(2):
                    rs = slice(ck * 64, (ck + 1) * 64)
                    for half in range(2):
                        bk = b0 if half == 0 else b1
                        bv = bk[0:64, :].bitcast(BF16)
                        for tl in range(2):
                            sT, sf = srcs[half * 2 + tl]
                            for h in range(8):
                                o_ = bv[:, (tl * 8 + h) * 64:(tl * 8 + h + 1) * 64]
                                p.tr(bk, o_, sf(rs, h), C['idb'][rs, rs], [sT, C['idb']])
                        eng = 'act' if half == 0 else 'dve'
                        p.cp(eng, FTS, FTS[:, ck, half * 1024:(half + 1) * 1024], bv, bk)
                p.dma('sp', None, G['ft_d'][2 * i:2 * i + 2].rearrange("c k n -> k c n"), FTS[:], FTS)
    p.es = G['es']
    p.barrier()


def phase2(p, C, G, l):
    hT, hTt, banks = G['hT'], G['hTt'], G['banks']
    W = G['W']
    b0, b1, b2, b3, b4, b5, b6, b7 = banks
    with ExitStack() as es:
        p.es = es
        qT = p.sb([128, 4, S], BF16, 'qT')
        kT = p.sb([128, 4, S], BF16, 'kT')
        p.dma('sp', qT, qT[:], G['qk_d'][0:4].rearrange("m p t -> p m t"))
        p.dma('sp', kT, kT[:], G['qk_d'][4:8].rearrange("m p t -> p m t"))
        wv = p.sb([128, 8, 512], BF16, 'wv')
        p.dma('pool', wv, wv[:], W['w_in'][l, :, 1024:1536].rearrange("(c p) f -> p c f", p=128))
        negs = p.sb([128, 128], BF16, 'negs')
        p.ms('pool', negs, negs[:], NEG)
        acc = p.sb([65, S], F32, 'acc')
        rden = p.sb([65, S], F32, 'rden')
        vaug = [p.sb([128, 32, 65], BF16, 'vaug') for _ in range(2)]
        for v_ in vaug:
            p.ms('pool', v_, v_[:, :, 64:65], 1.0)
        pT = [p.sb([128, 512], BF16, 'pT') for _ in range(2)]
        ao = [p.sb([64, S], BF16, 'ao') for _ in range(2)]
        idb, mb = C['idb'], C['mb']
        vi = 0
        si = 0
        for h in range(8):
            pb, pr = 64 * (h % 2), h // 2
            ps_ = slice(pb, pb + 64)
            for pi, d in enumerate([1, 4, 16]):
                nb = 32 // d
                va = vaug[vi % 2]
                vi += 1

                def tok(qb):
                    c, blk = divmod(qb, nb)
                    st = blk * 128 * d + c
                    return st, st + 127 * d + 1
                for kb8 in range(4):
                    bk = banks[kb8 % 2]
                    for j in range(8):
                        a_, e_ = tok(kb8 * 8 + j)
                        for c8 in range(8):
                            p.mm(bk, bk[:, j * 64:(j + 1) * 64], hT[:, c8, 2 + a_:2 + e_:d], wv[:, c8, h * 64:(h + 1) * 64],
                                 [wv, hTt[0]], start=(c8 == 0), stop=(c8 == 7))
                    eng = 'dve' if kb8 % 2 == 0 else 'act'
                    p.cp(eng, va, va[:, kb8 * 8:(kb8 + 1) * 8, 0:64], bk[:, :].rearrange("p (j e) -> p j e", j=8), bk)
                Ad = acc[:, :].rearrange("p (j dd) -> p dd j", dd=d)
                def emit_pv(q4, half, pt, qbs):
                    ob = b6 if q4 % 2 == 0 else b7
                    for qi, qb in enumerate(qbs):
                        blk = qb % nb
                        oo = ob[0:65, (half * 2 + qi) * 128:(half * 2 + qi + 1) * 128]
                        if blk > 0:
                            p.mm(ob, oo, va[:, qb - 1, :], pt[:, (qi * 2) * 128:(qi * 2 + 1) * 128], [va, pt],
                                 start=True, stop=False)
                        p.mm(ob, oo, va[:, qb, :], pt[:, (qi * 2 + 1) * 128:(qi * 2 + 2) * 128], [va, pt],
                             start=(blk == 0), stop=True)
                    if half == 1:
                        f0 = q4 * 512
                        per = S // d
                        if per >= 512:
                            av = Ad[:, f0 // per, f0 % per:f0 % per + 512]
                            ov = ob[0:65, :]
                        else:
                            ncl = 512 // per
                            av = Ad[:, f0 // per:f0 // per + ncl, :]
                            ov = ob[0:65, :].rearrange("p (c j) -> p c j", c=ncl)
                        if pi == 0:
                            p.cp('dve', acc, av, ov, ob)
                        else:
                            p.tt('dve', acc, av, av, ov, ALU.add, [acc, ob])

                pend = None
                for q4 in range(8):
                    for half in range(2):
                        sb_ = b4 if si % 2 == 0 else b5
                        pt = pT[si % 2]
                        si += 1
                        qbs = [q4 * 4 + half * 2, q4 * 4 + half * 2 + 1]
                        for qi, qb in enumerate(qbs):
                            blk = qb % nb
                            qa, qe = tok(qb)
                            qsl = qT[ps_, pr, qa:qe:d]
                            o_prev = sb_[:, (qi * 2) * 128:(qi * 2 + 1) * 128]
                            o_own = sb_[:, (qi * 2 + 1) * 128:(qi * 2 + 2) * 128]
                            if blk > 0:
                                ka, ke = tok(qb - 1)
                                p.mm(sb_, o_prev, kT[ps_, pr, ka:ke:d], qsl, [kT, qT], start=True, stop=False)
                                p.mm(sb_, o_prev, idb[:, :], mb[:, 0:128], [idb, mb], start=False, stop=True)
                            else:
                                p.mm(sb_, o_prev, idb[:, :], negs[:, :], [idb, negs], start=True, stop=True)
                            p.mm(sb_, o_own, kT[ps_, pr, qa:qe:d], qsl, [kT, qT], start=True, stop=False)
                            p.mm(sb_, o_own, idb[:, :], mb[:, 128:256], [idb, mb], start=False, stop=True)
                        p.act(pt, pt[:, :], sb_[:, :], AF.Exp, sb_)
                        if pend is not None:
                            emit_pv(*pend)
                        pend = (q4, half, pt, qbs)
                emit_pv(*pend)
            p.act(rden, rden[64:65, :], acc[64:65, :], AF.Ln, acc)
            p.act(rden, rden[64:65, :], rden[64:65, :], AF.Exp, rden, scale=-1.0)
            a_o = ao[h % 2]
            for g in range(8):
                bk = banks[g % 4]
                p.mm(bk, bk[0:64, :], C['ones'][64:65, 0:64], rden[64:65, g * 512:(g + 1) * 512], [C['ones'], rden])
                p.tt('dve', a_o, a_o[:, g * 512:(g + 1) * 512], acc[0:64, g * 512:(g + 1) * 512], bk[0:64, :], ALU.mult,
                     [acc, bk])
            p.dma('sp', None, G['att_d'][h * 64:(h + 1) * 64, :], a_o[:], a_o)
    p.es = G['es']
    p.barrier()


def phase3(p, C, G, l):
    banks = G['banks']
    b0, b1, b2, b3, b4, b5, b6, b7 = banks
    NCH = S // 64
    with ExitStack() as es:
        p.es = es
        FT = [p.sb([64, 2048], BF16, 'FT') for _ in range(2)]
        TM = [p.sb([64, 4, 512], BF16, 'TM') for _ in range(2)]
        GCt = [p.sb([64, 8], F32, 'GCt') for _ in range(2)]
        A = p.sb([64, 8, 64], F32, 'A')
        Abf = p.sb([64, 8, 64], BF16, 'Abf')
        TMP = p.sb([64, 8, 64], F32, 'TMP')
        p.ms('pool', A, A[:], 0.0)
        p.ms('pool', Abf, Abf[:], 0.0)
        f3 = lambda n, dt=F32: p.sb([64, 8, 64], dt, n)
        Pb = [f3('Pa'), f3('Pb')]
        Qb = [f3('Qa'), f3('Qb')]
        Zb = [f3('Za'), f3('Zb')]
        Zbf = f3('Zbf', BF16)
        Mrb, Mrk, Lak = f3('Mrb', BF16), f3('Mrk', BF16), f3('Lak', BF16)
        W1T, LV, Ubf = f3('W1T', BF16), f3('LV', BF16), f3('Ubf', BF16)
        W2 = f3('W2')
        Yo = [f3('Yo0'), f3('Yo1')]
        hv = lambda ap: ap.rearrange("p (h n) -> p h n", h=8)
        bc3 = lambda t: t[:].unsqueeze(1).to_broadcast([64, 8, 64])
        bc2 = lambda t: t[:].unsqueeze(1).to_broadcast([64, 4, 64])

        def load(c):
            b = c % 2
            p.dma('sp', FT[b], FT[b][:], G['ft_d'][c])
            p.dma('sp', TM[b], TM[b][:], G['tm_d'][c])
            p.dma('sp', GCt[b], GCt[b][:], G['gc_d'][c])

        load(0)
        for c in range(NCH):
            if c + 1 < NCH:
                load(c + 1)
            ft, tm, gc = FT[c % 2], TM[c % 2], GCt[c % 2]
            ftv = ft[:, :].rearrange("p (t h n) -> p t h n", t=4, h=8)
            tmh = lambda t, h: tm[:, t, h * 64:(h + 1) * 64]
            for h in range(8):
                aT, bT, kT_ = ftv[:, 0, h, :], ftv[:, 2, h, :], ftv[:, 3, h, :]
                arT = ftv[:, 0:2, h, :]
                p.mm(b0, b0[0:64, h * 64:(h + 1) * 64], aT, bT, [ft])
                bB = b1 if h < 4 else b2
                bK = b3 if h < 4 else b4
                o_ = slice((h % 4) * 128, (h % 4 + 1) * 128)
                p.mm(bB, bB[0:64, o_].rearrange("p (t n) -> p t n", t=2), bT, arT, [ft])
                p.mm(bK, bK[0:64, o_].rearrange("p (t n) -> p t n", t=2), kT_, arT, [ft])
            P0, Q0 = Pb[0], Qb[0]
            p.tt('dve', P0, P0[:], hv(b0[0:64, :]), bc3(C['mL']), ALU.mult, [b0, C['mL']])
            for half, (bB, bK) in enumerate([(b1, b3), (b2, b4)]):
                hs = slice(half * 4, half * 4 + 4)
                vB = bB[0:64, :].rearrange("p (h t n) -> p h t n", h=4, t=2)
                vK = bK[0:64, :].rearrange("p (h t n) -> p h t n", h=4, t=2)
                p.tt('dve', Q0, Q0[:, hs, :], vB[:, :, 0, :], bc2(C['mU']), ALU.mult, [bB, C['mU']])
                p.tt('dve', Mrb, Mrb[:, hs, :], vB[:, :, 1, :], bc2(C['mUI']), ALU.mult, [bB, C['mUI']])
                p.tt('dve', Lak, Lak[:, hs, :], vK[:, :, 0, :], bc2(C['mU']), ALU.mult, [bK, C['mU']])
                p.tt('dve', Mrk, Mrk[:, hs, :], vK[:, :, 1, :], bc2(C['mUI']), ALU.mult, [bK, C['mUI']])
            Zc = Zb[0]
            p.tt('dve', Zc, Zc[:], Q0[:], bc3_id(C), ALU.add, [Q0, C['idf']])
            Pc, Qc = P0, Q0
            for i in range(5):
                Pn, Qn, Zn = Pb[(i + 1) % 2], Qb[(i + 1) % 2], Zb[(i + 1) % 2]
                for h in range(8):
                    p.mm(b0, b0[0:64, h * 64:(h + 1) * 64], Qc[:, h, :], Pc[:, h, :], [Qc, Pc])
                if i < 4:
                    for h in range(8):
                        p.mm(b1, b1[0:64, h * 64:(h + 1) * 64], Pc[:, h, :], Qc[:, h, :], [Qc, Pc])
                p.cp('act', Pn, Pn[:], hv(b0[0:64, :]), b0)
                if i < 4:
                    p.cp('dve', Qn, Qn[:], hv(b1[0:64, :]), b1)
                for h in range(8):
                    p.mm(b2, b2[0:64, h * 64:(h + 1) * 64], Pn[:, h, :], Zc[:, h, :], [Pn, Zc])
                if i < 4:
                    p.tt('dve', Zn, Zn[:], hv(b2[0:64, :]), Zc[:], ALU.add, [b2, Zc])
                else:
                    p.tt('dve', Zbf, Zbf[:], hv(b2[0:64, :]), Zc[:], ALU.add, [b2, Zc])
                Pc, Qc, Zc = Pn, Qn, Zn
            for h in range(8):
                p.mm(b3, b3[0:64, h * 64:(h + 1) * 64], tmh(0, h), Zbf[:, h, :], [tm, Zbf])
            p.cp('act', W1T, W1T[:], hv(b3[0:64, :]), b3)
            for h in range(8):
                p.mm(b4, b4[0:64, h * 64:(h + 1) * 64], Lak[:, h, :], tmh(3, h), [tm, Lak])
            p.cp('dve', LV, LV[:], hv(b4[0:64, :]), b4)
            for h in range(8):
                p.mm(b3, b3[0:64, h * 64:(h + 1) * 64], Zbf[:, h, :], LV[:, h, :], [Zbf, LV])
            p.cp('act', W2, W2[:], hv(b3[0:64, :]), b3)
            for h in range(8):
                p.mm(b5, b5[0:64, h * 64:(h + 1) * 64], W1T[:, h, :], Abf[:, h, :], [W1T, Abf])
            p.tt('dve', Ubf, Ubf[:], hv(b5[0:64, :]), W2[:], ALU.add, [b5, W2])
            p.tt('dve', TMP, TMP[:], A[:], gc[:].unsqueeze(2).to_broadcast([64, 8, 64]), ALU.mult, [A, gc])
            for h in range(8):
                o_ = b6[0:64, h * 64:(h + 1) * 64]
                p.mm(b6, o_, ftv[:, 1, h, :], Abf[:, h, :], [ft, Abf], start=True, stop=False)
                p.mm(b6, o_, Mrb[:, h, :], Ubf[:, h, :], [Mrb, Ubf], start=False, stop=False)
                p.mm(b6, o_, Mrk[:, h, :], tmh(3, h), [Mrk, tm], start=False, stop=True)
            for h in range(8):
                o_ = b7[0:64, h * 64:(h + 1) * 64]
                p.mm(b7, o_, tmh(1, h), Ubf[:, h, :], [tm, Ubf], start=True, stop=False)
                p.mm(b7, o_, tmh(2, h), tmh(3, h), [tm], start=False, stop=True)
            p.tt('dve', A, A[:], TMP[:], hv(b7[0:64, :]), ALU.add, [TMP, b7])
            p.cp('act', Abf, Abf[:], A[:], A)
            yo = Yo[c % 2]
            p.cp('act', yo, yo[:], hv(b6[0:64, :]), b6)
            p.dma('sp', None, G['y_d'][c * 64:(c + 1) * 64, :].rearrange("s (h n) -> s h n", h=8), yo[:], yo)
    p.es = G['es']
    p.barrier()


def bc3_id(C):
    return C['idf'][0:64, 0:64].unsqueeze(1).to_broadcast([64, 8, 64])


def layer_norm_tile(p, z, outt, gbc, bbc, wk, eps):
    st, mv, rs = wk['st'], wk['mv'], wk['rs']
    for hf in range(2):
        p.add('dve', (lambda hf=hf: (lambda e: e.bn_stats(out=st[:, hf * 6:(hf + 1) * 6], in_=z[:, hf * 512:(hf + 1) * 512])))(),
              r=[z], w=[st])
    p.add('dve', lambda e: e.bn_aggr(out=mv[:], in_=st[:]), r=[st], w=[mv])
    p.act(rs, rs[:], mv[:, 1:2], AF.Sqrt, [mv, wk['eps']], bias=wk['eps'][:, 0:1])
    p.rcp(rs, rs[:], rs[:], rs)
    p.ts('dve', z, z[:], z[:], mv[:, 0:1], rs[:, 0:1], ALU.subtract, ALU.mult, [z, mv, rs])
    p.tt('dve', z, z[:], z[:], gbc[:], ALU.mult, [z, gbc])
    p.tt('dve', outt, outt[:], z[:], bbc[:], ALU.add, [z, bbc])


def phase4(p, C, G, l, xin):
    banks = G['banks']
    W = G['W']
    b0, b1, b2, b3, b4, b5, b6, b7 = banks
    moe = (l % 2 == 1)
    with ExitStack() as es:
        p.es = es
        wout = p.sb([128, 8, 1024], BF16, 'wout')
        p.dma('pool', wout, wout[:], W['w_out'][l].rearrange("(c p) f -> p c f", p=128))
        lg = p.sb([128, 512], F32, 'lg')
        lb = p.sb([128, 512], F32, 'lb')
        g1 = p.sb([128, 1024], F32, 'g1')
        b1_ = p.sb([128, 1024], F32, 'b1_')
        p.dma('sp', lg, lg[:], W['lnx_g'][l:l + 1, :].partition_broadcast(128))
        p.dma('sp', lb, lb[:], W['lnx_b'][l:l + 1, :].partition_broadcast(128))
        p.dma('sp', g1, g1[:], W['ln1_g'][l:l + 1, :].partition_broadcast(128))
        p.dma('sp', b1_, b1_[:], W['ln1_b'][l:l + 1, :].partition_broadcast(128))
        if moe:
            rt = p.sb([128, 8, 8], F32, 'rt')
            p.dma('sp', rt, rt[:], W['router'][l // 2].rearrange("(c p) e -> p c e", p=128))
        wk = dict(st=p.sb([128, 12], F32, 'st'), mv=p.sb([128, 2], F32, 'mv'), rs=p.sb([128, 1], F32, 'rs'),
                  eps=p.sb([128, 1], F32, 'eps'))
        p.ms('pool', wk['eps'], wk['eps'][:], LN_EPS)
        geps = p.sb([128, 1], F32, 'geps')
        p.ms('pool', geps, geps[:], GN_EPS)
        f = lambda n: p.sb([128, 512], F32, n)
        Y = [f('Y0'), f('Y1')]
        V = [f('V0'), f('V1')]
        Gt = [f('G0'), f('G1')]
        RK = [p.sb([128, 8], F32, 'RK') for _ in range(2)]
        X = [p.sb([128, 1024], F32, 'X') for _ in range(2)]
        AT = [p.sb([128, 4, 128], BF16, 'AT') for _ in range(2)]
        SQ = f('SQ')
        RW = p.sb([128, 512], BF16, 'RW')
        RWT = p.sb([128, 4, 128], BF16, 'RWT')
        Z = p.sb([128, 1024], F32, 'Z')
        X1 = [p.sb([128, 1024], F32, 'X1') for _ in range(2)]
        X1T = [p.sb([128, 8, 128], BF16, 'X1T') for _ in range(2)]
        X1F = p.sb([128, 8, 128], F32, 'X1F')
        s1, s2, mean, msq, var, rstd = [p.sb([128, 8], F32, n) for n in ['s1', 's2', 'mean', 'msq', 'var', 'rstd']]
        lgt, top8, msk, ex, cmb = [p.sb([128, 8], F32, n) for n in ['lgt', 'top8', 'msk', 'ex', 'cmb']]
        nm1, den = p.sb([128, 1], F32, 'nm1'), p.sb([128, 1], F32, 'den')
        h8 = lambda ap: ap.rearrange("p (h k) -> p h k", h=8)
        b8 = lambda t: t[:].unsqueeze(2).to_broadcast([128, 8, 64])

        def load(i):
            b = i % 2
            t0 = i * 128
            p.dma('sp', Y[b], Y[b][:], G['y_d'][t0:t0 + 128, :])
            p.dma('sp', V[b], V[b][:], G['v_d'][t0:t0 + 128, :])
            p.dma('sp', Gt[b], Gt[b][:], G['g_d'][t0:t0 + 128, :])
            p.dma('sp', RK[b], RK[b][:], G['rk_d'][t0:t0 + 128, :])
            p.dma('sp', X[b], X[b][:], xin[t0:t0 + 128, :])
            p.dma('sp', AT[b], AT[b][:], G['att_d'][:, t0:t0 + 128].rearrange("(c p) t -> p c t", p=128))

        load(0)
        for i in range(NT):
            if i + 1 < NT:
                load(i + 1)
            b = i % 2
            t0 = i * 128
            y, v, g, rk, x, at = Y[b], V[b], Gt[b], RK[b], X[b], AT[b]
            p.red(s1, s1[:], h8(y[:]), y)
            p.tt('dve', SQ, SQ[:], y[:], y[:], ALU.mult, [y])
            p.red(s2, s2[:], h8(SQ[:]), SQ)
            p.ts('dve', mean, mean[:], s1[:], 1.0 / 64, None, ALU.mult, None, s1)
            p.tt('dve', msq, msq[:], mean[:], mean[:], ALU.mult, [mean])
            p.stt(var, var[:], s2[:], 1.0 / 64, msq[:], ALU.mult, ALU.subtract, [s2, msq])
            p.act(rstd, rstd[:], var[:], AF.Sqrt, [var, geps], bias=geps[:, 0:1])
            p.rcp(rstd, rstd[:], rstd[:], rstd)
            p.tt('dve', y, h8(y[:]), h8(y[:]), b8(mean), ALU.subtract, [y, mean])
            p.tt('dve', y, h8(y[:]), h8(y[:]), b8(rstd), ALU.mult, [y, rstd])
            p.tt('dve', y, y[:], y[:], lg[:], ALU.mult, [y, lg])
            p.tt('dve', y, y[:], y[:], lb[:], ALU.add, [y, lb])
            p.tt('dve', SQ, h8(SQ[:]), h8(v[:]), b8(rk), ALU.mult, [v, rk])
            p.tt('dve', y, y[:], y[:], SQ[:], ALU.add, [y, SQ])
            p.tt('dve', RW, RW[:], y[:], g[:], ALU.mult, [y, g])
            bv = b0[:, :].bitcast(BF16)
            for c in range(4):
                p.tr(b0, bv[:, c * 128:(c + 1) * 128], RW[:, c * 128:(c + 1) * 128], C['idb'][:], [RW, C['idb']])
            p.cp('act', RWT, RWT[:], bv[:, 0:512].rearrange("p (c t) -> p c t", c=4), b0)
            for dc, bk in enumerate([b1, b2]):
                cs = slice(dc * 512, (dc + 1) * 512)
                for c in range(4):
                    p.mm(bk, bk[:, :], at[:, c, :], wout[:, c, cs], [at, wout], start=(c == 0), stop=False)
                for c in range(4):
                    p.mm(bk, bk[:, :], RWT[:, c, :], wout[:, 4 + c, cs], [RWT, wout], start=False, stop=(c == 3))
                p.stt(Z, Z[:, cs], x[:, cs], ALPHA, bk[:, :], ALU.mult, ALU.add, [x, bk])
            x1 = X1[b]
            layer_norm_tile(p, Z, x1, g1, b1_, wk, LN_EPS)
            p.dma('sp', None, G['x1_d'][t0:t0 + 128, :], x1[:], x1)
            x1t = X1T[b]
            for hb, bk in enumerate([b3, b4]):
                for cc in range(4):
                    c = hb * 4 + cc
                    p.tr(bk, bk[:, cc * 128:(cc + 1) * 128], x1[:, c * 128:(c + 1) * 128], C['idf'][:], [x1, C['idf']])
                if moe:
                    p.cp('dve', X1F, X1F[:, hb * 4:(hb + 1) * 4, :], bk[:, :].rearrange("p (c t) -> p c t", c=4), bk)
                    p.cp('act', x1t, x1t[:, hb * 4:(hb + 1) * 4, :], X1F[:, hb * 4:(hb + 1) * 4, :], X1F)
                else:
                    p.cp('act', x1t, x1t[:, hb * 4:(hb + 1) * 4, :], bk[:, :].rearrange("p (c t) -> p c t", c=4), bk)
            p.dma('sp', None, G['x1T_d'][:, :, t0:t0 + 128], x1t[:], x1t)
            if moe and RMODE >= 2:
                for c in range(8):
                    p.mm(b5, b5[:, 0:8], X1F[:, c, :], rt[:, c, :], [X1F, rt], start=(c == 0), stop=(c == 7))
                p.cp('dve', lgt, lgt[:], b5[:, 0:8], b5)
            if moe and RMODE >= 3:
                p.add('dve', lambda e: e.max(out=top8[:], in_=lgt[:]), r=[lgt], w=[top8])
                p.ts('dve', msk, msk[:], lgt[:], top8[:, 1:2], None, ALU.is_ge, None, [lgt, top8])
                p.ts('dve', nm1, nm1[:], top8[:, 0:1], -1.0, None, ALU.mult, None, top8)
                p.act(ex, ex[:], lgt[:], AF.Exp, [lgt, nm1], bias=nm1[:, 0:1])
                p.tt('dve', ex, ex[:], ex[:], msk[:], ALU.mult, [ex, msk])
                p.red(den, den[:], ex[:], ex)
                p.rcp(den, den[:], den[:], den)
                p.ts('dve', cmb, cmb[:], ex[:], den[:, 0:1], None, ALU.mult, None, [ex, den])
                p.dma('sp', None, G['comb_d'][t0:t0 + 128, :], cmb[:], cmb)
    p.es = G['es']
    p.barrier()


def phase5(p, C, G, l, xout):
    banks = G['banks']
    W = G['W']
    moe = (l % 2 == 1)
    li = l // 2
    HT = S // 2
    NTH = HT // 128
    if moe:
        experts = list(range(NE))
        fcs = [(k * 512, 512) for k in range(DFE // 512)]
        wg_ = lambda e: W['moe_w_gate'][li, e]
        wu_ = lambda e: W['moe_w_up'][li, e]
        wd_ = lambda e: W['moe_w_down'][li, e]
    else:
        experts = [0]
        fcs = [(k * 512, 512) for k in range(DFF // 512)] + [(DFF - DFF % 512, DFF % 512)]
        wg_ = lambda e: W['ffn_w_gate'][li]
        wu_ = lambda e: W['ffn_w_up'][li]
        wd_ = lambda e: W['ffn_w_down'][li]
    with ExitStack() as es:
        p.es = es
        g2 = p.sb([128, 1024], F32, 'g2')
        b2_ = p.sb([128, 1024], F32, 'b2_')
        p.dma('sp', g2, g2[:], W['ln2_g'][l:l + 1, :].partition_broadcast(128))
        p.dma('sp', b2_, b2_[:], W['ln2_b'][l:l + 1, :].partition_broadcast(128))
        wk = dict(st=p.sb([128, 12], F32, 'st'), mv=p.sb([128, 2], F32, 'mv'), rs=p.sb([128, 1], F32, 'rs'),
                  eps=p.sb([128, 1], F32, 'eps'))
        p.ms('pool', wk['eps'], wk['eps'][:], LN_EPS)
        xT = p.sb([128, 8, HT], BF16, 'xT')
        yacc = p.sb([128, NTH, 1024], F32, 'yacc')
        yt = [T(yacc.h, 'yacc%d' % i) for i in range(NTH)]
        comb = p.sb([128, NTH, 8], F32, 'comb')
        WG = [p.sb([128, 8, 512], BF16, 'WG') for _ in range(2)]
        WU = [p.sb([128, 8, 512], BF16, 'WU') for _ in range(2)]
        WD = [p.sb([128, 4, 1024], BF16, 'WD') for _ in range(2)]
        H1 = [p.sb([128, 4, 512], BF16, 'H1') for _ in range(2)]
        SG = [p.sb([128, 512], F32, 'SG') for _ in range(2)]
        OUT = [p.sb([128, 1024], F32, 'OUT') for _ in range(2)]
        units = [(e, fc) for e in experts for fc in fcs]

        def loadgu(ui):
            e, (f0, fs) = units[ui]
            b = ui % 2
            p.dma('pool', WG[b], WG[b][:, :, 0:fs], wg_(e)[:, f0:f0 + fs].rearrange("(c p) f -> p c f", p=128))
            p.dma('pool', WU[b], WU[b][:, :, 0:fs], wu_(e)[:, f0:f0 + fs].rearrange("(c p) f -> p c f", p=128))

        def loadd(ui):
            e, (f0, fs) = units[ui]
            b = ui % 2
            nft = fs // 128
            p.dma('pool', WD[b], WD[b][:, 0:nft, :], wd_(e)[f0:f0 + fs, :].rearrange("(c p) d -> p c d", p=128))

        state = dict(hi=0, gi=0, yi=0)

        def stage_a(ui, tg):
            e, (f0, fs) = units[ui]
            wg, wu = WG[ui % 2], WU[ui % 2]
            nft = fs // 128
            h1 = H1[state['hi'] % 2]
            state['hi'] += 1
            tsl = slice(tg * 512, (tg + 1) * 512)
            for ft in range(nft):
                gi = state['gi']
                pg = banks[(gi % 2) * 2]
                pu = banks[(gi % 2) * 2 + 1]
                sg = SG[gi % 2]
                state['gi'] += 1
                fsl = slice(ft * 128, (ft + 1) * 128)
                for c in range(8):
                    p.mm(pg, pg[:, :], wg[:, c, fsl], xT[:, c, tsl], [wg, xT], start=(c == 0), stop=(c == 7))
                for c in range(8):
                    p.mm(pu, pu[:, :], wu[:, c, fsl], xT[:, c, tsl], [wu, xT], start=(c == 0), stop=(c == 7))
                p.act(sg, sg[:], pg[:, :], AF.Silu, pg)
                p.tt('dve', h1, h1[:, ft, :], sg[:], pu[:, :], ALU.mult, [sg, pu])
            return (ui, tg, h1)

        def stage_b(ui, tg, h1):
            e, (f0, fs) = units[ui]
            wd = WD[ui % 2]
            nft = fs // 128
            for tt_ in range(4):
                ti = tg * 4 + tt_
                for dc in range(2):
                    py = banks[4 + (state['yi'] % 4)]
                    state['yi'] += 1
                    cs = slice(dc * 512, (dc + 1) * 512)
                    for ft in range(nft):
                        p.mm(py, py[:, :], h1[:, ft, tt_ * 128:(tt_ + 1) * 128], wd[:, ft, cs], [h1, wd],
                             start=(ft == 0), stop=(ft == nft - 1))
                    if moe:
                        p.stt(yt[ti], yacc[:, ti, cs], py[:, :], comb[:, ti, e:e + 1], yacc[:, ti, cs],
                              ALU.mult, ALU.add, [py, comb, yt[ti]])
                    else:
                        p.tt('dve', yt[ti], yacc[:, ti, cs], yacc[:, ti, cs], py[:, :], ALU.add, [yt[ti], py])

        NTG = HT // 512
        for half in range(2):
            tb = half * HT
            p.dma('sp', xT, xT[:], G['x1T_d'][:, :, tb:tb + HT])
            for i in range(NTH):
                p.dma('sp', yt[i], yacc[:, i, :], G['x1_d'][tb + i * 128:tb + (i + 1) * 128, :])
                p.ts('dve' if i % 2 == 0 else 'pool', yt[i], yacc[:, i, :], yacc[:, i, :], ALPHA, None, ALU.mult, None, yt[i])
            if moe:
                p.dma('sp', comb, comb[:], G['comb_d'][tb:tb + HT, :].rearrange("(i p) e -> p i e", p=128))
            loadgu(0)
            loadd(0)
            pend = None
            for ui in range(len(units)):
                for tg in range(NTG):
                    cur = stage_a(ui, tg)
                    if pend is not None:
                        stage_b(*pend)
                    pend = cur
                    if tg == 0 and ui + 1 < len(units):
                        loadgu(ui + 1)
                        loadd(ui + 1)
            stage_b(*pend)
            for i in range(NTH):
                o = OUT[i % 2]
                st, mv, rs = wk['st'], wk['mv'], wk['rs']
                z = yacc[:, i, :]
                for hf in range(2):
                    p.add('dve', (lambda hf=hf, i=i: (lambda en: en.bn_stats(out=st[:, hf * 6:(hf + 1) * 6],
                                                                           in_=yacc[:, i, hf * 512:(hf + 1) * 512])))(),
                          r=[yt[i]], w=[st])
                p.add('dve', lambda en: en.bn_aggr(out=mv[:], in_=st[:]), r=[st], w=[mv])
                p.act(rs, rs[:], mv[:, 1:2], AF.Sqrt, [mv, wk['eps']], bias=wk['eps'][:, 0:1])
                p.rcp(rs, rs[:], rs[:], rs)
                p.ts('dve', yt[i], z, z, mv[:, 0:1], rs[:, 0:1], ALU.subtract, ALU.mult, [yt[i], mv, rs])
                p.tt('dve', yt[i], z, z, g2[:], ALU.mult, [yt[i], g2])
                p.tt('dve', o, o[:], z, b2_[:], ALU.add, [yt[i], b2_])
                p.dma('sp', None, xout[tb + i * 128:tb + (i + 1) * 128, :], o[:], o)
    p.es = G['es']
    p.barrier()


WNAMES = ['w_in', 'w_in_vres', 'shift_mu', 'shift_mu_vres', 'decay_up', 'decay_base', 'aaa_up', 'aaa_base',
          'vres_up', 'vres_base', 'gate_up', 'k_k', 'k_a', 'r_k', 'lnx_g', 'lnx_b', 'w_out', 'ln1_g', 'ln1_b',
          'ln2_g', 'ln2_b', 'ffn_w_gate', 'ffn_w_up', 'ffn_w_down', 'router', 'moe_w_gate', 'moe_w_up', 'moe_w_down']


def build_program(shapes, nlayers=DEPTH, stop_phase=None, dbg=(), layers=None):
    nc = bass.Bass("TRN2", target_bir_lowering=False)
    x = nc.dram_tensor("x", [S, D], F32, kind="ExternalInput").ap()
    W = {n: nc.dram_tensor(n, list(shapes[n]), F32, kind="ExternalInput").ap() for n in WNAMES}
    out = nc.dram_tensor("out", [S, D], F32, kind="ExternalOutput").ap()

    def scr(name, shape, dt):
        kind = "ExternalOutput" if name in dbg else "Internal"
        return nc.dram_tensor(name, shape, dt, kind=kind).ap()

    G = {'W': W}
    G['qk_d'] = scr('qk_d', [8, 128, S], BF16)
    G['att_d'] = scr('att_d', [512, S], BF16)
    G['g_d'] = scr('g_d', [S, 512], F32)
    G['v_d'] = scr('v_d', [S, 512], F32)
    G['vfirst_d'] = scr('vfirst_d', [S, 512], F32)
    G['rk_d'] = scr('rk_d', [S, 8], F32)
    G['y_d'] = scr('y_d', [S, 512], F32)
    G['gc_d'] = scr('gc_d', [S // 64, 64, 8], F32)
    G['tm_d'] = scr('tm_d', [S // 64, 64, 4, 512], BF16)
    G['ft_d'] = scr('ft_d', [S // 64, 64, 2048], BF16)
    G['x1_d'] = scr('x1_d', [S, D], F32)
    G['x1T_d'] = scr('x1T_d', [128, 8, S], BF16)
    G['comb_d'] = scr('comb_d', [S, 8], F32)
    G['xcur_d'] = scr('xcur_d', [S, D], F32)
    with ExitStack() as es:
        p = Prog(nc, es)
        G['es'] = es
        C = build_consts(p)
        G['banks'] = [p.ps([128, 512], F32, 'bank%d' % i) for i in range(8)]
        p.barrier()
        done = False
        lay = list(range(nlayers)) if layers is None else list(layers)
        for l in lay:
            xin = x if l == lay[0] else G['xcur_d']
            xout = out if (l == lay[-1] and stop_phase is None) else G['xcur_d']
            phases = [lambda: phase1a(p, C, G, l, xin), lambda: phase1b(p, C, G, l), lambda: phase2(p, C, G, l),
                      lambda: phase3(p, C, G, l), lambda: phase4(p, C, G, l, xin), lambda: phase5(p, C, G, l, xout)]
            with ExitStack() as esf:
                p.es = esf
                G['es'] = esf
                hT = p.sb([128, 8, S + 2], BF16, 'hT')
                G['hT'] = hT
                G['hTt'] = [T(hT.h, 'hT%d' % i) for i in range(NT)]
                p.ms('pool', G['hTt'][0], hT[:, :, 0:2], 0.0)
                for pi in range(3):
                    phases[pi]()
                    if stop_phase is not None and (l, pi) == tuple(stop_phase):
                        done = True
                        break
            p.es = es
            G['es'] = es
            if not done:
                for pi in range(3, 6):
                    phases[pi]()
                    if stop_phase is not None and (l, pi) == tuple(stop_phase):
                        done = True
                        break
            if done:
                break
        if done:
            with ExitStack() as es2:
                p.es = es2
                tmp = p.sb([128, 1024], F32, 'dbgtmp')
                p.dma('sp', tmp, tmp[:], x[0:128, :])
                p.dma('sp', None, out[0:128, :], tmp[:], tmp)
            p.es = es
            p.barrier()
        stats = p.emit()
    return nc, stats


_CACHE = {}


def kernel(**inputs):
    x = np.ascontiguousarray(inputs['x'], dtype=np.float32)
    B = x.shape[0]
    shapes = {n: inputs[n].shape for n in WNAMES}
    if 'nc' not in _CACHE:
        _CACHE['nc'] = build_program(shapes)[0]
    nc = _CACHE['nc']
    wmap = {n: np.ascontiguousarray(inputs[n], dtype=np.float32) for n in WNAMES}
    in_maps = []
    for b in range(B):
        m = dict(wmap)
        m['x'] = x[b]
        in_maps.append(m)
    res = run_bass_kernel_spmd(nc, in_maps, core_ids=list(range(B)))
    return np.stack([np.asarray(r['out'], dtype=np.float32).reshape(S, D) for r in res.results], axis=0)
```

```python
import numpy as np
import concourse.bass as bass
import concourse.mybir as mybir
from concourse.bass_utils import run_bass_kernel_spmd
from contextlib import ExitStack

F32 = mybir.dt.float32
BF16 = mybir.dt.bfloat16
AF = mybir.ActivationFunctionType
ALU = mybir.AluOpType
AX = mybir.AxisListType

S = 4096
D = 1024
DEPTH = 4
NT = S // 128
ALPHA = float((2 * DEPTH) ** 0.25)
LN_EPS = 1e-5
GN_EPS = 64e-5
CDEC = float(np.exp(-0.5))
NEG = -30000.0
DFF = 2816
DFE = 3584
NE = 8

import os
RMODE = int(os.environ.get('RMODE', '3'))
SEM_CH = 12000
N_ESEM = 12
N_DSEM = 24


class T:
    __slots__ = ('h', 'name', 'lastw', 'rd_eng', 'rd_dma')

    ALL = []

    def __init__(self, h, name=''):
        self.h = h
        self.name = name
        self.lastw = None
        self.rd_eng = {}
        self.rd_dma = []
        T.ALL.append(self)

    def __getitem__(self, k):
        return self.h[k]


class Op:
    __slots__ = ('eng', 'fn', 'deps', 'is_dma', 'marked', 'done', 'slot_prev')

    def __init__(self, eng, fn, is_dma):
        self.eng = eng
        self.fn = fn
        self.deps = []
        self.is_dma = is_dma
        self.marked = is_dma
        self.done = None
        self.slot_prev = None


class Prog:
    ENGS = ['pe', 'act', 'dve', 'pool', 'sp']

    def __init__(self, nc, es):
        T.ALL.clear()
        self.nc = nc
        self.es = es
        self.ops = {e: [] for e in self.ENGS}
        self.esem = {e: [es.enter_context(nc.semaphore(f's_{e}_{i}')) for i in range(N_ESEM)]
                     for e in self.ENGS}
        self.dsem = [es.enter_context(nc.semaphore(f'd_{i}')) for i in range(N_DSEM)]
        self.dcount = [0] * N_DSEM
        self.dnext = 0
        self.dma_since_barrier = []
        self.n_tiles = 0

    def sb(self, shape, dtype, name=None):
        self.n_tiles += 1
        name = name or 't'
        h = self.es.enter_context(self.nc.sbuf_tensor(f'{name}_{self.n_tiles}', list(shape), dtype))
        return T(h, name)

    def ps(self, shape, dtype=F32, name=None):
        self.n_tiles += 1
        name = name or 'p'
        h = self.es.enter_context(self.nc.psum_tensor(f'{name}_{self.n_tiles}', list(shape), dtype))
        return T(h, name)

    def add(self, eng, fn, r=(), w=(), dma=False):
        op = Op(eng, fn, dma)
        deps = []
        for t in r:
            if t is None:
                continue
            if t.lastw is not None:
                deps.append(t.lastw)
        for t in w:
            if t is None:
                continue
            if t.lastw is not None:
                deps.append(t.lastw)
            deps.extend(t.rd_eng.values())
            deps.extend(t.rd_dma)
        seen = set()
        for d in deps:
            if d is op or id(d) in seen:
                continue
            seen.add(id(d))
            if (not d.is_dma) and (not dma) and d.eng == eng and eng == 'pe':
                continue
            d.marked = True
            op.deps.append(d)
        for t in r:
            if t is None:
                continue
            if dma:
                t.rd_dma.append(op)
            else:
                t.rd_eng[eng] = op
        for t in w:
            if t is None:
                continue
            t.lastw = op
            t.rd_eng = {}
            t.rd_dma = []
        if dma:
            s = self.dnext
            self.dnext = (self.dnext + 1) % N_DSEM
            op.slot_prev = (s, self.dcount[s])
            self.dcount[s] += 16
            op.done = (self.dsem[s], self.dcount[s])
            self.dma_since_barrier.append(op)
        self.ops[eng].append(op)
        return op

    def barrier(self):
        lasts = []
        for e in self.ENGS:
            for o in reversed(self.ops[e]):
                if not o.is_dma:
                    lasts.append(o)
                    break
        dmas = list(self.dma_since_barrier)
        self.dma_since_barrier = []
        for e in self.ENGS:
            op = Op(e, lambda eng: eng.nop(), False)
            for d in lasts + dmas:
                d.marked = True
                op.deps.append(d)
            self.ops[e].append(op)
        for t in T.ALL:
            t.lastw = None
            t.rd_eng = {}
            t.rd_dma = []

    def emit(self):
        nc = self.nc
        for e in self.ENGS:
            n = 0
            for o in self.ops[e]:
                if o.is_dma:
                    continue
                if o.marked:
                    n += 1
                    si = (n - 1) // SEM_CH
                    assert si < N_ESEM, f'too many marked ops on {e}: {n}'
                    o.done = (self.esem[e][si], (n - 1) % SEM_CH + 1)
        engmap = {'pe': 'tensor', 'act': 'scalar', 'dve': 'vector', 'pool': 'gpsimd', 'sp': 'sync'}
        with nc.Block() as block:
            for e in self.ENGS:
                ops = self.ops[e]

                def body(eng, ops=ops):
                    waited = {}
                    for o in ops:
                        ws = [d.done for d in o.deps]
                        if o.is_dma and o.slot_prev[1] > 0:
                            ws.append((self.dsem[o.slot_prev[0]], o.slot_prev[1]))
                        for (sem, val) in ws:
                            k = id(sem)
                            if waited.get(k, 0) >= val:
                                continue
                            waited[k] = val
                            eng.wait_ge(sem, val)
                        inst = o.fn(eng)
                        if o.is_dma:
                            inst.then_inc(o.done[0], 16)
                        elif o.marked:
                            inst.then_inc(o.done[0], 1)

                getattr(block, engmap[e])(body)
        return {e: len(self.ops[e]) for e in self.ENGS}

    @staticmethod
    def _l(x):
        if x is None:
            return []
        return list(x) if isinstance(x, (list, tuple)) else [x]

    def mm(self, wT, o, l, r, rd, start=True, stop=True):
        return self.add('pe', lambda e: e.matmul(o, lhsT=l, rhs=r, start=start, stop=stop), r=self._l(rd), w=self._l(wT))

    def tr(self, wT, o, i, idn, rd):
        return self.add('pe', lambda e: e.transpose(o, i, idn), r=self._l(rd), w=self._l(wT))

    def act(self, wT, o, i, func, rd, scale=None, bias=None):
        kw = {}
        if scale is not None:
            kw['scale'] = scale
        if bias is not None:
            kw['bias'] = bias
        return self.add('act', lambda e: e.activation(out=o, in_=i, func=func, **kw), r=self._l(rd), w=self._l(wT))

    def tt(self, eng, wT, o, a, b, op, rd):
        return self.add(eng, lambda e: e.tensor_tensor(out=o, in0=a, in1=b, op=op), r=self._l(rd), w=self._l(wT))

    def ts(self, eng, wT, o, a, s1, s2, op0, op1, rd):
        if op1 is None:
            return self.add(eng, lambda e: e.tensor_scalar(out=o, in0=a, scalar1=s1, scalar2=None, op0=op0),
                            r=self._l(rd), w=self._l(wT))
        return self.add(eng, lambda e: e.tensor_scalar(out=o, in0=a, scalar1=s1, scalar2=s2, op0=op0, op1=op1),
                        r=self._l(rd), w=self._l(wT))

    def stt(self, wT, o, a, sc, b, op0, op1, rd):
        return self.add('dve', lambda e: e.scalar_tensor_tensor(out=o, in0=a, scalar=sc, in1=b, op0=op0, op1=op1),
                        r=self._l(rd), w=self._l(wT))

    def red(self, wT, o, i, rd, op=None):
        op = op or ALU.add
        return self.add('dve', lambda e: e.tensor_reduce(out=o, in_=i, axis=AX.X, op=op), r=self._l(rd), w=self._l(wT))

    def rcp(self, wT, o, i, rd):
        return self.add('dve', lambda e: e.reciprocal(out=o, in_=i), r=self._l(rd), w=self._l(wT))

    def cp(self, eng, wT, o, i, rd):
        if eng == 'act':
            return self.add('act', lambda e: e.activation(out=o, in_=i, func=AF.Copy), r=self._l(rd), w=self._l(wT))
        return self.add(eng, lambda e: e.tensor_copy(out=o, in_=i), r=self._l(rd), w=self._l(wT))

    def ms(self, eng, wT, o, val):
        return self.add(eng, lambda e: e.memset(o, val), w=self._l(wT))

    def dma(self, eng, wT, o, i, rd=None):
        return self.add(eng, lambda e: e.dma_start(out=o, in_=i), r=self._l(rd), w=self._l(wT), dma=True)

    def asel(self, wT, o, i, pattern, cmp, fill, base, cm, rd):
        return self.add('pool', lambda e: e.affine_select(out=o, in_=i, pattern=pattern, compare_op=cmp, fill=fill,
                                                          base=base, channel_multiplier=cm), r=self._l(rd), w=self._l(wT))


def build_consts(p):
    C = {}
    ones = p.sb([128, 128], F32, 'ones')
    zeros = p.sb([128, 128], F32, 'zeros')
    p.ms('pool', ones, ones[:], 1.0)
    p.ms('pool', zeros, zeros[:], 0.0)
    idf = p.sb([128, 128], F32, 'idf')
    p.asel(idf, idf[:], ones[:], [[-1, 128]], ALU.is_equal, 0.0, 0, 1, ones)
    idb = p.sb([128, 128], BF16, 'idb')
    p.cp('pool', idb, idb[:], idf[:], idf)
    mbo32 = p.sb([128, 128], F32, 'mbo32')
    mbp32 = p.sb([128, 128], F32, 'mbp32')
    p.asel(mbo32, mbo32[:], zeros[:], [[1, 128]], ALU.is_ge, NEG, 0, -1, zeros)
    p.asel(mbp32, mbp32[:], zeros[:], [[-1, 128]], ALU.is_ge, NEG, 0, 1, zeros)
    mb = p.sb([128, 256], BF16, 'mb')
    p.cp('pool', mb, mb[:, 0:128], mbp32[:], mbp32)
    p.cp('pool', mb, mb[:, 128:256], mbo32[:], mbo32)
    mL = p.sb([64, 64], F32, 'mL')
    mU = p.sb([64, 64], F32, 'mU')
    mUI = p.sb([64, 64], F32, 'mUI')
    p.asel(mL, mL[:], ones[0:64, 0:64], [[-1, 64]], ALU.is_gt, 0.0, 0, 1, ones)
    p.asel(mU, mU[:], ones[0:64, 0:64], [[1, 64]], ALU.is_gt, 0.0, 0, -1, ones)
    p.asel(mUI, mUI[:], ones[0:64, 0:64], [[1, 64]], ALU.is_ge, 0.0, 0, -1, ones)
    tri = p.sb([128, 128], F32, 'tri')
    p.asel(tri, tri[:], ones[:], [[1, 128]], ALU.is_ge, 0.0, 0, -1, ones)
    p.ms('pool', tri, tri[0:64, 64:128], 0.0)
    tot = p.sb([128, 128], F32, 'tot')
    p.ms('pool', tot, tot[:], 0.0)
    p.ms('pool', tot, tot[0:64, 0:64], 1.0)
    p.ms('pool', tot, tot[64:128, 64:128], 1.0)
    cind = p.sb([128, 2], F32, 'cind')
    p.ms('pool', cind, cind[:], 0.0)
    p.ms('pool', cind, cind[0:64, 0:1], 1.0)
    p.ms('pool', cind, cind[64:128, 1:2], 1.0)
    onesb = p.sb([128, 64], BF16, 'onesb')
    p.ms('pool', onesb, onesb[:], 1.0)
    C.update(ones=ones, zeros=zeros, idf=idf, idb=idb, mb=mb, mL=mL, mU=mU, mUI=mUI, tri=tri, tot=tot,
             cind=cind, onesb=onesb)
    return C


def phase1a(p, C, G, l, xin):
    hT, hTt, banks = G['hT'], G['hTt'], G['banks']
    W = G['W']
    with ExitStack() as es:
        p.es = es
        wqk = p.sb([128, 8, 1024], BF16, 'wqk')
        p.dma('pool', wqk, wqk[:], W['w_in'][l, :, 0:1024].rearrange("(c p) f -> p c f", p=128))
        xt = [p.sb([128, 1024], F32, 'xt') for _ in range(2)]
        qks = [p.sb([128, 8, 512], BF16, 'qks') for _ in range(2)]
        for i in range(NT):
            x_ = xt[i % 2]
            p.dma('sp', x_, x_[:], xin[i * 128:(i + 1) * 128, :])
            for hb in range(2):
                bk = banks[hb]
                for cc in range(4):
                    c = hb * 4 + cc
                    p.tr(bk, bk[:, cc * 128:(cc + 1) * 128], x_[:, c * 128:(c + 1) * 128], C['idf'][:], [x_, C['idf']])
                eng = 'act' if hb == 0 else 'dve'
                p.cp(eng, hTt[i], hT[:, hb * 4:(hb + 1) * 4, 2 + i * 128:2 + (i + 1) * 128],
                     bk[:, :].rearrange("p (c t) -> p c t", c=4), bk)
            if i % 4 == 3:
                g = i // 4
                st = qks[g % 2]
                rd = [hTt[j] for j in range(g * 4, g * 4 + 4)]
                for m in range(8):
                    bk = banks[2 + (m % 2)]
                    for c in range(8):
                        p.mm(bk, bk[:, :], wqk[:, c, m * 128:(m + 1) * 128],
                             hT[:, c, 2 + g * 512:2 + (g + 1) * 512], [wqk] + rd, start=(c == 0), stop=(c == 7))
                    sc = 0.125 if m < 4 else 1.0
                    p.act(st, st[:, m, :], bk[:, :], AF.Identity, bk, scale=sc)
                p.dma('sp', None, G['qk_d'][:, :, g * 512:(g + 1) * 512].rearrange("m p t -> p m t"), st[:], st)
    p.es = G['es']
    p.barrier()


def phase1b(p, C, G, l):
    hT, hTt, banks = G['hT'], G['hTt'], G['banks']
    W = G['W']
    NW = 1792 + (32 if l > 0 else 0)
    b0, b1, b2, b3, b4, b5, b6, b7 = banks
    with ExitStack() as es:
        p.es = es
        wa = p.sb([128, 8, 1824], BF16, 'wa')
        wb = p.sb([128, 8, 1824], BF16, 'wb')
        with ExitStack() as es2:
            p.es = es2
            mu = p.sb([128, 1824], F32, 'mu')
            omu = p.sb([128, 1824], F32, 'omu')
            p.dma('sp', mu, mu[:, 0:1792], W['shift_mu'][l:l + 1, :].partition_broadcast(128))
            if l > 0:
                p.dma('sp', mu, mu[:, 1792:1824], W['shift_mu_vres'][l - 1:l, :].partition_broadcast(128))
            p.ts('dve', omu, omu[:, 0:NW], mu[:, 0:NW], -1.0, 1.0, ALU.mult, ALU.add, mu)
            stg = [p.sb([128, 1824], F32, 'stg') for _ in range(2)]
            for c in range(8):
                s_ = stg[c % 2]
                p.dma('sp', s_, s_[:, 0:1792], W['w_in'][l, c * 128:(c + 1) * 128, 1536:3328])
                if l > 0:
                    p.dma('sp', s_, s_[:, 1792:1824], W['w_in_vres'][l - 1, c * 128:(c + 1) * 128, :])
                p.tt('dve', wa, wa[:, c, 0:NW], s_[:, 0:NW], omu[:, 0:NW], ALU.mult, [s_, omu])
                p.tt('pool', wb, wb[:, c, 0:NW], s_[:, 0:NW], mu[:, 0:NW], ALU.mult, [s_, mu])
            p.barrier()
        p.es = es
        names = ['decay_base', 'aaa_base', 'k_k', 'k_a', 'r_k']
        bc = {}
        for n in names:
            bc[n] = p.sb([128, 512], F32, 'bc_' + n)
            p.dma('sp', bc[n], bc[n][:], W[n][l:l + 1, :].partition_broadcast(128))
        if l > 0:
            bc['vres_base'] = p.sb([128, 512], F32, 'bc_vb')
            p.dma('sp', bc['vres_base'], bc['vres_base'][:], W['vres_base'][l - 1:l, :].partition_broadcast(128))
        lup1 = p.sb([128, 512], BF16, 'lup1')
        p.dma('pool', lup1, lup1[0:64, :], W['decay_up'][l])
        p.dma('pool', lup1, lup1[64:128, :], W['aaa_up'][l])
        gup = p.sb([128, 512], BF16, 'gup')
        p.dma('pool', gup, gup[:], W['gate_up'][l])
        if l > 0:
            vup = p.sb([32, 512], BF16, 'vup')
            p.dma('pool', vup, vup[:], W['vres_up'][l - 1])
        f = lambda n: p.sb([128, 512], F32, n)
        T1, T2, SW, AA, GG, VV, VF, KK, KP, BB, CS = [f(n) for n in
                                                      ['T1', 'T2', 'SW', 'AA', 'GG', 'VV', 'VF', 'KK', 'KP', 'BB', 'CS']]
        TMS = p.sb([128, 4, 512], BF16, 'TMS')
        RT = p.sb([128, 512], BF16, 'RT')
        BT = p.sb([128, 512], BF16, 'BT')
        KT = p.sb([128, 512], BF16, 'KT')
        FTS = p.sb([64, 2, 2048], BF16, 'FTS')
        GC = p.sb([64, 2, 8], F32, 'GC')
        sm = lambda n, w_: p.sb([128, w_], F32, n)
        ssq, rn, rkb = sm('ssq', 8), sm('rn', 8), sm('rkb', 8)
        lo1 = p.sb([128, 512], BF16, 'lo1')
        lo2 = p.sb([128, 512], BF16, 'lo2')
        lo3 = p.sb([32, 512], BF16, 'lo3')
        h8 = lambda ap: ap.rearrange("p (h k) -> p h k", h=8)

        for g in range(NT // 4):
            rd = [hTt[j] for j in range(max(0, g * 4 - 1), g * 4 + 4)]
            specs = [(1536, 128, lo1, b0), (1664, 128, lo2, b1)]
            if l > 0:
                specs.append((1792, 32, lo3, b0))
            for (c0, wd_, lo, bk) in specs:
                for c in range(8):
                    p.mm(bk, bk[0:wd_, :], wa[:, c, c0:c0 + wd_], hT[:, c, 2 + g * 512:2 + (g + 1) * 512],
                         [wa] + rd, start=(c == 0), stop=False)
                    p.mm(bk, bk[0:wd_, :], wb[:, c, c0:c0 + wd_], hT[:, c, 1 + g * 512:1 + (g + 1) * 512],
                         [wb] + rd, start=False, stop=(c == 7))
                if lo is lo1:
                    p.act(lo, lo[0:64, :], bk[0:64, :], AF.Tanh, bk)
                    p.cp('dve', lo, lo[64:128, :], bk[64:128, :], bk)
                elif lo is lo2:
                    p.act(lo, lo[:, :], bk[:, :], AF.Sigmoid, bk)
                else:
                    p.cp('dve', lo, lo[0:32, :], bk[0:32, :], bk)
            for ti in range(4):
                i = g * 4 + ti
                t0 = i * 128
                ts_ = slice(ti * 128, (ti + 1) * 128)
                rdh = [hTt[j] for j in range(max(0, i - 1), i + 1)]
                if l > 0:
                    p.dma('sp', VF, VF[:], G['vfirst_d'][t0:t0 + 128, :])
                for n, bk in enumerate([b2, b3, b4]):
                    for c in range(8):
                        p.mm(bk, bk[:, :], hT[:, c, 2 + t0:2 + t0 + 128], wa[:, c, n * 512:(n + 1) * 512],
                             [wa] + rdh, start=(c == 0), stop=False)
                        p.mm(bk, bk[:, :], hT[:, c, 1 + t0:1 + t0 + 128], wb[:, c, n * 512:(n + 1) * 512],
                             [wb] + rdh, start=False, stop=(c == 7))
                p.mm(b5, b5[:, :], lo1[0:64, ts_], lup1[0:64, :], [lo1, lup1])
                p.mm(b6, b6[:, :], lo1[64:128, ts_], lup1[64:128, :], [lo1, lup1])
                p.mm(b7, b7[:, :], lo2[:, ts_], gup[:, :], [lo2, gup])
                p.tt('dve', T1, T1[:], b5[:, :], bc['decay_base'][:], ALU.add, [b5, bc['decay_base']])
                p.act(SW, SW[:], T1[:], AF.Sigmoid, T1)
                p.tt('dve', T2, T2[:], b6[:, :], bc['aaa_base'][:], ALU.add, [b6, bc['aaa_base']])
                p.act(AA, AA[:], T2[:], AF.Sigmoid, T2)
                p.cp('act', GG, GG[:], b7[:, :], b7)
                p.dma('sp', None, G['g_d'][t0:t0 + 128, :], GG[:], GG)
                if l == 0:
                    p.cp('act', VV, VV[:], b4[:, :], b4)
                    p.dma('sp', None, G['vfirst_d'][t0:t0 + 128, :], VV[:], VV)
                else:
                    p.mm(b0, b0[:, :], lo3[0:32, ts_], vup[0:32, :], [lo3, vup])
                    p.tt('dve', T2, T2[:], b0[:, :], bc['vres_base'][:], ALU.add, [b0, bc['vres_base']])
                    p.act(T2, T2[:], T2[:], AF.Sigmoid, T2)
                    p.tt('dve', VV, VV[:], VF[:], b4[:, :], ALU.subtract, [VF, b4])
                    p.tt('dve', VV, VV[:], VV[:], T2[:], ALU.mult, [VV, T2])
                    p.tt('dve', VV, VV[:], VV[:], b4[:, :], ALU.add, [VV, b4])
                p.dma('sp', None, G['v_d'][t0:t0 + 128, :], VV[:], VV)
                p.cp('act', TMS, TMS[:, 3, :], VV[:], VV)
                p.tt('dve', KK, KK[:], b3[:, :], bc['k_k'][:], ALU.mult, [b3, bc['k_k']])
                p.tt('dve', T1, T1[:], KK[:], KK[:], ALU.mult, [KK])
                p.red(ssq, ssq[:], h8(T1[:]), T1)
                p.act(rn, rn[:], ssq[:], AF.Sqrt, ssq)
                p.ts('dve', rn, rn[:], rn[:], 1e-12, None, ALU.max, None, rn)
                p.rcp(rn, rn[:], rn[:], rn)
                p.tt('dve', KK, h8(KK[:]), h8(KK[:]), rn[:].unsqueeze(2).to_broadcast([128, 8, 64]), ALU.mult, [KK, rn])
                p.stt(T1, T1[:], AA[:], -1.0, bc['k_a'][:], ALU.add, ALU.mult, [AA, bc['k_a']])
                p.stt(KP, KP[:], T1[:], 1.0, b3[:, :], ALU.add, ALU.mult, [T1, b3])
                p.tt('dve', BB, BB[:], KK[:], AA[:], ALU.mult, [KK, AA])
                p.tt('dve', T1, T1[:], b2[:, :], bc['r_k'][:], ALU.mult, [b2, bc['r_k']])
                p.tt('dve', T1, T1[:], T1[:], KP[:], ALU.mult, [T1, KP])
                p.red(rkb, rkb[:], h8(T1[:]), T1)
                p.dma('sp', None, G['rk_d'][t0:t0 + 128, :], rkb[:], rkb)
                p.mm(b5, b5[:, :], C['tri'][:], SW[:], [C['tri'], SW])
                p.mm(b6, b6[:, :], C['tot'][:], SW[:], [C['tot'], SW])
                gcv = b7[0:64, 0:16].rearrange("p (c h) -> p c h", c=2)
                for h in range(8):
                    p.mm(b7, gcv[:, :, h], SW[:, h * 64:(h + 1) * 64], C['cind'][:], [SW, C['cind']])
                p.act(GC, GC[:], gcv, AF.Exp, b7, scale=-CDEC)
                p.dma('sp', None, G['gc_d'][2 * i:2 * i + 2].rearrange("c k h -> k c h"), GC[:], GC)
                p.cp('act', CS, CS[:], b5[:, :], b5)
                p.act(T1, T1[:], CS[:], AF.Exp, CS, scale=-CDEC)
                p.tt('dve', RT, RT[:], b2[:, :], T1[:], ALU.mult, [b2, T1])
                p.tt('dve', T1, T1[:], CS[:], SW[:], ALU.subtract, [CS, SW])
                p.act(T1, T1[:], T1[:], AF.Exp, T1, scale=-CDEC)
                p.stt(TMS, TMS[:, 0, :], KK[:], -1.0, T1[:], ALU.mult, ALU.mult, [KK, T1])
                p.act(T1, T1[:], CS[:], AF.Exp, CS, scale=CDEC)
                p.tt('dve', BT, BT[:], BB[:], T1[:], ALU.mult, [BB, T1])
                p.tt('dve', KT, KT[:], KP[:], T1[:], ALU.mult, [KP, T1])
                p.tt('dve', T2, T2[:], b6[:, :], CS[:], ALU.subtract, [b6, CS])
                p.act(T2, T2[:], T2[:], AF.Exp, T2, scale=-CDEC)
                p.tt('dve', TMS, TMS[:, 1, :], BB[:], T2[:], ALU.mult, [BB, T2])
                p.tt('dve', TMS, TMS[:, 2, :], KP[:], T2[:], ALU.mult, [KP, T2])
                p.dma('sp', None, G['tm_d'][2 * i:2 * i + 2].rearrange("c s t f -> (c s) t f"), TMS[:], TMS)
                srcs = [(TMS, lambda r_, h_: TMS[r_, 0, h_ * 64:(h_ + 1) * 64]),
                        (RT, lambda r_, h_: RT[r_, h_ * 64:(h_ + 1) * 64]),
                        (BT, lambda r_, h_: BT[r_, h_ * 64:(h_ + 1) * 64]),
                        (KT, lambda r_, h_: KT[r_, h_ * 64:(h_ + 1) * 64])]
                for ck in range(2):
                    rs = slice(ck * 64, (ck + 1) * 64)
                    for half in range(2):
                        bk = b0 if half == 0 else b1
                        bv = bk[0:64, :].bitcast(BF16)
                        for tl in range(2):
                            sT, sf = srcs[half * 2 + tl]
                            for h in range(8):
                                o_ = bv[:, (tl * 8 + h) * 64:(tl * 8 + h + 1) * 64]
                                p.tr(bk, o_, sf(rs, h), C['idb'][rs, rs], [sT, C['idb']])
                        eng = 'act' if half == 0 else 'dve'
                        p.cp(eng, FTS, FTS[:, ck, half * 1024:(half + 1) * 1024], bv, bk)
                p.dma('sp', None, G['ft_d'][2 * i:2 * i + 2].rearrange("c k n -> k c n"), FTS[:], FTS)
    p.es = G['es']
    p.barrier()


def phase2(p, C, G, l):
    hT, hTt, banks = G['hT'], G['hTt'], G['banks']
    W = G['W']
    b0, b1, b2, b3, b4, b5, b6, b7 = banks
    with ExitStack() as es:
        p.es = es
        qT = p.sb([128, 4, S], BF16, 'qT')
        kT = p.sb([128, 4, S], BF16, 'kT')
        p.dma('sp', qT, qT[:], G['qk_d'][0:4].rearrange("m p t -> p m t"))
        p.dma('sp', kT, kT[:], G['qk_d'][4:8].rearrange("m p t -> p m t"))
        wv = p.sb([128, 8, 512], BF16, 'wv')
        p.dma('pool', wv, wv[:], W['w_in'][l, :, 1024:1536].rearrange("(c p) f -> p c f", p=128))
        negs = p.sb([128, 128], BF16, 'negs')
        p.ms('pool', negs, negs[:], NEG)
        acc = p.sb([65, S], F32, 'acc')
        rden = p.sb([65, S], F32, 'rden')
        vaug = [p.sb([128, 32, 65], BF16, 'vaug') for _ in range(2)]
        for v_ in vaug:
            p.ms('pool', v_, v_[:, :, 64:65], 1.0)
        pT = [p.sb([128, 512], BF16, 'pT') for _ in range(2)]
        ao = [p.sb([64, S], BF16, 'ao') for _ in range(2)]
        idb, mb = C['idb'], C['mb']
        vi = 0
        si = 0
        for h in range(8):
            pb, pr = 64 * (h % 2), h // 2
            ps_ = slice(pb, pb + 64)
            for pi, d in enumerate([1, 4, 16]):
                nb = 32 // d
                va = vaug[vi % 2]
                vi += 1

                def tok(qb):
                    c, blk = divmod(qb, nb)
                    st = blk * 128 * d + c
                    return st, st + 127 * d + 1
                for kb8 in range(4):
                    bk = banks[kb8 % 2]
                    for j in range(8):
                        a_, e_ = tok(kb8 * 8 + j)
                        for c8 in range(8):
                            p.mm(bk, bk[:, j * 64:(j + 1) * 64], hT[:, c8, 2 + a_:2 + e_:d], wv[:, c8, h * 64:(h + 1) * 64],
                                 [wv, hTt[0]], start=(c8 == 0), stop=(c8 == 7))
                    eng = 'dve' if kb8 % 2 == 0 else 'act'
                    p.cp(eng, va, va[:, kb8 * 8:(kb8 + 1) * 8, 0:64], bk[:, :].rearrange("p (j e) -> p j e", j=8), bk)
                Ad = acc[:, :].rearrange("p (j dd) -> p dd j", dd=d)
                def emit_pv(q4, half, pt, qbs):
                    ob = b6 if q4 % 2 == 0 else b7
                    for qi, qb in enumerate(qbs):
                        blk = qb % nb
                        oo = ob[0:65, (half * 2 + qi) * 128:(half * 2 + qi + 1) * 128]
                        if blk > 0:
                            p.mm(ob, oo, va[:, qb - 1, :], pt[:, (qi * 2) * 128:(qi * 2 + 1) * 128], [va, pt],
                                 start=True, stop=False)
                        p.mm(ob, oo, va[:, qb, :], pt[:, (qi * 2 + 1) * 128:(qi * 2 + 2) * 128], [va, pt],
                             start=(blk == 0), stop=True)
                    if half == 1:
                        f0 = q4 * 512
                        per = S // d
                        if per >= 512:
                            av = Ad[:, f0 // per, f0 % per:f0 % per + 512]
                            ov = ob[0:65, :]
                        else:
                            ncl = 512 // per
                            av = Ad[:, f0 // per:f0 // per + ncl, :]
                            ov = ob[0:65, :].rearrange("p (c j) -> p c j", c=ncl)
                        if pi == 0:
                            p.cp('dve', acc, av, ov, ob)
                        else:
                            p.tt('dve', acc, av, av, ov, ALU.add, [acc, ob])

                pend = None
                for q4 in range(8):
                    for half in range(2):
                        sb_ = b4 if si % 2 == 0 else b5
                        pt = pT[si % 2]
                        si += 1
                        qbs = [q4 * 4 + half * 2, q4 * 4 + half * 2 + 1]
                        for qi, qb in enumerate(qbs):
                            blk = qb % nb
                            qa, qe = tok(qb)
                            qsl = qT[ps_, pr, qa:qe:d]
                            o_prev = sb_[:, (qi * 2) * 128:(qi * 2 + 1) * 128]
                            o_own = sb_[:, (qi * 2 + 1) * 128:(qi * 2 + 2) * 128]
                            if blk > 0:
                                ka, ke = tok(qb - 1)
                                p.mm(sb_, o_prev, kT[ps_, pr, ka:ke:d], qsl, [kT, qT], start=True, stop=False)
                                p.mm(sb_, o_prev, idb[:, :], mb[:, 0:128], [idb, mb], start=False, stop=True)
                            else:
                                p.mm(sb_, o_prev, idb[:, :], negs[:, :], [idb, negs], start=True, stop=True)
                            p.mm(sb_, o_own, kT[ps_, pr, qa:qe:d], qsl, [kT, qT], start=True, stop=False)
                            p.mm(sb_, o_own, idb[:, :], mb[:, 128:256], [idb, mb], start=False, stop=True)
                        p.act(pt, pt[:, :], sb_[:, :], AF.Exp, sb_)
                        if pend is not None:
                            emit_pv(*pend)
                        pend = (q4, half, pt, qbs)
                emit_pv(*pend)
            p.act(rden, rden[64:65, :], acc[64:65, :], AF.Ln, acc)
            p.act(rden, rden[64:65, :], rden[64:65, :], AF.Exp, rden, scale=-1.0)
            a_o = ao[h % 2]
            for g in range(8):
                bk = banks[g % 4]
                p.mm(bk, bk[0:64, :], C['ones'][64:65, 0:64], rden[64:65, g * 512:(g + 1) * 512], [C['ones'], rden])
                p.tt('dve', a_o, a_o[:, g * 512:(g + 1) * 512], acc[0:64, g * 512:(g + 1) * 512], bk[0:64, :], ALU.mult,
                     [acc, bk])
            p.dma('sp', None, G['att_d'][h * 64:(h + 1) * 64, :], a_o[:], a_o)
    p.es = G['es']
    p.barrier()


def phase3_group(p, C, G, bk4, h0, FT, TM, GCt):
    NH = 4
    bA, bB, bC, bD = bk4
    A = p.sb([64, NH, 64], F32, 'A')
    Abf = p.sb([64, NH, 64], BF16, 'Abf')
    TMP = p.sb([64, NH, 64], F32, 'TMP')
    p.ms('pool', A, A[:], 0.0)
    p.ms('pool', Abf, Abf[:], 0.0)
    f3 = lambda n, dt=F32: p.sb([64, NH, 64], dt, n)
    Pb = [f3('Pa'), f3('Pb')]
    Qb = [f3('Qa'), f3('Qb')]
    Zb = [f3('Za'), f3('Zb')]
    Zbf = f3('Zbf', BF16)
    Mrb, Mrk, Lak = f3('Mrb', BF16), f3('Mrk', BF16), f3('Lak', BF16)
    W1T, LV, Ubf = f3('W1T', BF16), f3('LV', BF16), f3('Ubf', BF16)
    W2 = f3('W2')
    Yo = [f3('Yo0'), f3('Yo1')]
    NW = NH * 64
    hv = lambda bk: bk[0:64, 0:NW].rearrange("p (h n) -> p h n", h=NH)
    bc = lambda t: t[:].unsqueeze(1).to_broadcast([64, NH, 64])
    idb4 = C['idf'][0:64, 0:64].unsqueeze(1).to_broadcast([64, NH, 64])

    def chunk(c):
        ft, tm, gc = FT[c % 2], TM[c % 2], GCt[c % 2]
        ftv = ft[:, :].rearrange("p (t h n) -> p t h n", t=4, h=8)
        tmh = lambda t, h: tm[:, t, h * 64:(h + 1) * 64]
        hs = [(hh, h0 + hh) for hh in range(NH)]
        col = lambda hh: slice(hh * 64, (hh + 1) * 64)
        col2 = lambda hh: slice(hh * 128, (hh + 1) * 128)
        for hh, h in hs:
            aT, bT = ftv[:, 0, h, :], ftv[:, 2, h, :]
            arT = ftv[:, 0:2, h, :]
            p.mm(bA, bA[0:64, col(hh)], aT, bT, [ft])
            p.mm(bB, bB[0:64, col2(hh)].rearrange("p (t n) -> p t n", t=2), bT, arT, [ft])
        yield
        P0, Q0 = Pb[0], Qb[0]
        p.tt('dve', P0, P0[:], hv(bA), bc(C['mL']), ALU.mult, [bA, C['mL']])
        vB = bB[0:64, :].rearrange("p (h t n) -> p h t n", h=NH, t=2)
        p.tt('dve', Q0, Q0[:], vB[:, :, 0, :], bc(C['mU']), ALU.mult, [bB, C['mU']])
        Zc = Zb[0]
        p.tt('dve', Zc, Zc[:], Q0[:], idb4, ALU.add, [Q0, C['idf']])
        p.tt('dve', Mrb, Mrb[:], vB[:, :, 1, :], bc(C['mUI']), ALU.mult, [bB, C['mUI']])
        for hh, h in hs:
            kT_ = ftv[:, 3, h, :]
            arT = ftv[:, 0:2, h, :]
            p.mm(bC, bC[0:64, col2(hh)].rearrange("p (t n) -> p t n", t=2), kT_, arT, [ft])
        Pc, Qc = P0, Q0
        for i in range(5):
            Pn, Qn, Zn = Pb[(i + 1) % 2], Qb[(i + 1) % 2], Zb[(i + 1) % 2]
            for hh, h in hs:
                p.mm(bA, bA[0:64, col(hh)], Qc[:, hh, :], Pc[:, hh, :], [Qc, Pc])
            if i < 4:
                for hh, h in hs:
                    p.mm(bB, bB[0:64, col(hh)], Pc[:, hh, :], Qc[:, hh, :], [Qc, Pc])
            yield
            if i == 0:
                vK = bC[0:64, :].rearrange("p (h t n) -> p h t n", h=NH, t=2)
                p.tt('dve', Lak, Lak[:], vK[:, :, 0, :], bc(C['mU']), ALU.mult, [bC, C['mU']])
                p.tt('dve', Mrk, Mrk[:], vK[:, :, 1, :], bc(C['mUI']), ALU.mult, [bC, C['mUI']])
            p.cp('act', Pn, Pn[:], hv(bA), bA)
            if i < 4:
                p.cp('dve', Qn, Qn[:], hv(bB), bB)
            for hh, h in hs:
                p.mm(bD, bD[0:64, col(hh)], Pn[:, hh, :], Zc[:, hh, :], [Pn, Zc])
            yield
            if i < 4:
                p.tt('dve', Zn, Zn[:], hv(bD), Zc[:], ALU.add, [bD, Zc])
            else:
                p.tt('dve', Zbf, Zbf[:], hv(bD), Zc[:], ALU.add, [bD, Zc])
            Pc, Qc, Zc = Pn, Qn, Zn
        for hh, h in hs:
            p.mm(bC, bC[0:64, col(hh)], tmh(0, h), Zbf[:, hh, :], [tm, Zbf])
        for hh, h in hs:
            p.mm(bA, bA[0:64, col(hh)], Lak[:, hh, :], tmh(3, h), [tm, Lak])
        yield
        p.cp('act', W1T, W1T[:], hv(bC), bC)
        p.cp('dve', LV, LV[:], hv(bA), bA)
        for hh, h in hs:
            p.mm(bB, bB[0:64, col(hh)], Zbf[:, hh, :], LV[:, hh, :], [Zbf, LV])
        yield
        p.cp('act', W2, W2[:], hv(bB), bB)
        for hh, h in hs:
            p.mm(bD, bD[0:64, col(hh)], W1T[:, hh, :], Abf[:, hh, :], [W1T, Abf])
        yield
        p.tt('dve', Ubf, Ubf[:], hv(bD), W2[:], ALU.add, [bD, W2])
        p.tt('dve', TMP, TMP[:], A[:], gc[:, h0:h0 + NH].unsqueeze(2).to_broadcast([64, NH, 64]), ALU.mult, [A, gc])
        for hh, h in hs:
            o_ = bC[0:64, col(hh)]
            p.mm(bC, o_, tmh(1, h), Ubf[:, hh, :], [tm, Ubf], start=True, stop=False)
            p.mm(bC, o_, tmh(2, h), tmh(3, h), [tm], start=False, stop=True)
        for hh, h in hs:
            o_ = bA[0:64, col(hh)]
            p.mm(bA, o_, ftv[:, 1, h, :], Abf[:, hh, :], [ft, Abf], start=True, stop=False)
            p.mm(bA, o_, Mrb[:, hh, :], Ubf[:, hh, :], [Mrb, Ubf], start=False, stop=False)
            p.mm(bA, o_, Mrk[:, hh, :], tmh(3, h), [Mrk, tm], start=False, stop=True)
        yield
        p.tt('dve', A, A[:], TMP[:], hv(bC), ALU.add, [TMP, bC])
        p.cp('act', Abf, Abf[:], A[:], A)
        yo = Yo[c % 2]
        p.cp('act', yo, yo[:], hv(bA), bA)
        p.dma('sp', None, G['y_d'][c * 64:(c + 1) * 64, h0 * 64:(h0 + NH) * 64].rearrange("s (h n) -> s h n", h=NH),
              yo[:], yo)

    return chunk


def phase3(p, C, G, l):
    banks = G['banks']
    NCH = S // 64
    with ExitStack() as es:
        p.es = es
        FT = [p.sb([64, 2048], BF16, 'FT') for _ in range(2)]
        TM = [p.sb([64, 4, 512], BF16, 'TM') for _ in range(2)]
        GCt = [p.sb([64, 8], F32, 'GCt') for _ in range(2)]
        groups = [phase3_group(p, C, G, banks[0:4], 0, FT, TM, GCt),
                  phase3_group(p, C, G, banks[4:8], 4, FT, TM, GCt)]

        def load(c):
            b = c % 2
            p.dma('sp', FT[b], FT[b][:], G['ft_d'][c])
            p.dma('sp', TM[b], TM[b][:], G['tm_d'][c])
            p.dma('sp', GCt[b], GCt[b][:], G['gc_d'][c])

        load(0)
        for c in range(NCH):
            if c + 1 < NCH:
                load(c + 1)
            gens = [g(c) for g in groups]
            while gens:
                for g in list(gens):
                    try:
                        next(g)
                    except StopIteration:
                        gens.remove(g)
    p.es = G['es']
    p.barrier()


def bc3_id(C):
    return C['idf'][0:64, 0:64].unsqueeze(1).to_broadcast([64, 8, 64])


def layer_norm_tile(p, z, outt, gbc, bbc, wk, eps):
    st, mv, rs = wk['st'], wk['mv'], wk['rs']
    for hf in range(2):
        p.add('dve', (lambda hf=hf: (lambda e: e.bn_stats(out=st[:, hf * 6:(hf + 1) * 6], in_=z[:, hf * 512:(hf + 1) * 512])))(),
              r=[z], w=[st])
    p.add('dve', lambda e: e.bn_aggr(out=mv[:], in_=st[:]), r=[st], w=[mv])
    p.act(rs, rs[:], mv[:, 1:2], AF.Sqrt, [mv, wk['eps']], bias=wk['eps'][:, 0:1])
    p.rcp(rs, rs[:], rs[:], rs)
    p.ts('dve', z, z[:], z[:], mv[:, 0:1], rs[:, 0:1], ALU.subtract, ALU.mult, [z, mv, rs])
    p.tt('dve', z, z[:], z[:], gbc[:], ALU.mult, [z, gbc])
    p.tt('dve', outt, outt[:], z[:], bbc[:], ALU.add, [z, bbc])


def phase4(p, C, G, l, xin):
    banks = G['banks']
    W = G['W']
    b0, b1, b2, b3, b4, b5, b6, b7 = banks
    moe = (l % 2 == 1)
    with ExitStack() as es:
        p.es = es
        wout = p.sb([128, 8, 1024], BF16, 'wout')
        p.dma('pool', wout, wout[:], W['w_out'][l].rearrange("(c p) f -> p c f", p=128))
        lg = p.sb([128, 512], F32, 'lg')
        lb = p.sb([128, 512], F32, 'lb')
        g1 = p.sb([128, 1024], F32, 'g1')
        b1_ = p.sb([128, 1024], F32, 'b1_')
        p.dma('sp', lg, lg[:], W['lnx_g'][l:l + 1, :].partition_broadcast(128))
        p.dma('sp', lb, lb[:], W['lnx_b'][l:l + 1, :].partition_broadcast(128))
        p.dma('sp', g1, g1[:], W['ln1_g'][l:l + 1, :].partition_broadcast(128))
        p.dma('sp', b1_, b1_[:], W['ln1_b'][l:l + 1, :].partition_broadcast(128))
        if moe:
            rt = p.sb([128, 8, 8], F32, 'rt')
            p.dma('sp', rt, rt[:], W['router'][l // 2].rearrange("(c p) e -> p c e", p=128))
        wk = dict(st=p.sb([128, 12], F32, 'st'), mv=p.sb([128, 2], F32, 'mv'), rs=p.sb([128, 1], F32, 'rs'),
                  eps=p.sb([128, 1], F32, 'eps'))
        p.ms('pool', wk['eps'], wk['eps'][:], LN_EPS)
        geps = p.sb([128, 1], F32, 'geps')
        p.ms('pool', geps, geps[:], GN_EPS)
        f = lambda n: p.sb([128, 512], F32, n)
        Y = [f('Y0'), f('Y1')]
        V = [f('V0'), f('V1')]
        Gt = [f('G0'), f('G1')]
        RK = [p.sb([128, 8], F32, 'RK') for _ in range(2)]
        X = [p.sb([128, 1024], F32, 'X') for _ in range(2)]
        AT = [p.sb([128, 4, 128], BF16, 'AT') for _ in range(2)]
        SQ = f('SQ')
        RW = p.sb([128, 512], BF16, 'RW')
        RWT = p.sb([128, 4, 128], BF16, 'RWT')
        Z = p.sb([128, 1024], F32, 'Z')
        X1 = [p.sb([128, 1024], F32, 'X1') for _ in range(2)]
        X1T = [p.sb([128, 8, 128], BF16, 'X1T') for _ in range(2)]
        X1F = p.sb([128, 8, 128], F32, 'X1F')
        s1, s2, mean, msq, var, rstd = [p.sb([128, 8], F32, n) for n in ['s1', 's2', 'mean', 'msq', 'var', 'rstd']]
        lgt, top8, msk, ex, cmb = [p.sb([128, 8], F32, n) for n in ['lgt', 'top8', 'msk', 'ex', 'cmb']]
        nm1, den = p.sb([128, 1], F32, 'nm1'), p.sb([128, 1], F32, 'den')
        h8 = lambda ap: ap.rearrange("p (h k) -> p h k", h=8)
        b8 = lambda t: t[:].unsqueeze(2).to_broadcast([128, 8, 64])

        def load(i):
            b = i % 2
            t0 = i * 128
            p.dma('sp', Y[b], Y[b][:], G['y_d'][t0:t0 + 128, :])
            p.dma('sp', V[b], V[b][:], G['v_d'][t0:t0 + 128, :])
            p.dma('sp', Gt[b], Gt[b][:], G['g_d'][t0:t0 + 128, :])
            p.dma('sp', RK[b], RK[b][:], G['rk_d'][t0:t0 + 128, :])
            p.dma('sp', X[b], X[b][:], xin[t0:t0 + 128, :])
            p.dma('sp', AT[b], AT[b][:], G['att_d'][:, t0:t0 + 128].rearrange("(c p) t -> p c t", p=128))

        load(0)
        for i in range(NT):
            if i + 1 < NT:
                load(i + 1)
            b = i % 2
            t0 = i * 128
            y, v, g, rk, x, at = Y[b], V[b], Gt[b], RK[b], X[b], AT[b]
            p.red(s1, s1[:], h8(y[:]), y)
            p.tt('dve', SQ, SQ[:], y[:], y[:], ALU.mult, [y])
            p.red(s2, s2[:], h8(SQ[:]), SQ)
            p.ts('dve', mean, mean[:], s1[:], 1.0 / 64, None, ALU.mult, None, s1)
            p.tt('dve', msq, msq[:], mean[:], mean[:], ALU.mult, [mean])
            p.stt(var, var[:], s2[:], 1.0 / 64, msq[:], ALU.mult, ALU.subtract, [s2, msq])
            p.act(rstd, rstd[:], var[:], AF.Sqrt, [var, geps], bias=geps[:, 0:1])
            p.rcp(rstd, rstd[:], rstd[:], rstd)
            p.tt('dve', y, h8(y[:]), h8(y[:]), b8(mean), ALU.subtract, [y, mean])
            p.tt('dve', y, h8(y[:]), h8(y[:]), b8(rstd), ALU.mult, [y, rstd])
            p.tt('dve', y, y[:], y[:], lg[:], ALU.mult, [y, lg])
            p.tt('dve', y, y[:], y[:], lb[:], ALU.add, [y, lb])
            p.tt('dve', SQ, h8(SQ[:]), h8(v[:]), b8(rk), ALU.mult, [v, rk])
            p.tt('dve', y, y[:], y[:], SQ[:], ALU.add, [y, SQ])
            p.tt('dve', RW, RW[:], y[:], g[:], ALU.mult, [y, g])
            bv = b0[:, :].bitcast(BF16)
            for c in range(4):
                p.tr(b0, bv[:, c * 128:(c + 1) * 128], RW[:, c * 128:(c + 1) * 128], C['idb'][:], [RW, C['idb']])
            p.cp('act', RWT, RWT[:], bv[:, 0:512].rearrange("p (c t) -> p c t", c=4), b0)
            for dc, bk in enumerate([b1, b2]):
                cs = slice(dc * 512, (dc + 1) * 512)
                for c in range(4):
                    p.mm(bk, bk[:, :], at[:, c, :], wout[:, c, cs], [at, wout], start=(c == 0), stop=False)
                for c in range(4):
                    p.mm(bk, bk[:, :], RWT[:, c, :], wout[:, 4 + c, cs], [RWT, wout], start=False, stop=(c == 3))
                p.stt(Z, Z[:, cs], x[:, cs], ALPHA, bk[:, :], ALU.mult, ALU.add, [x, bk])
            x1 = X1[b]
            layer_norm_tile(p, Z, x1, g1, b1_, wk, LN_EPS)
            p.dma('sp', None, G['x1_d'][t0:t0 + 128, :], x1[:], x1)
            x1t = X1T[b]
            for hb, bk in enumerate([b3, b4]):
                for cc in range(4):
                    c = hb * 4 + cc
                    p.tr(bk, bk[:, cc * 128:(cc + 1) * 128], x1[:, c * 128:(c + 1) * 128], C['idf'][:], [x1, C['idf']])
                if moe:
                    p.cp('dve', X1F, X1F[:, hb * 4:(hb + 1) * 4, :], bk[:, :].rearrange("p (c t) -> p c t", c=4), bk)
                    p.cp('act', x1t, x1t[:, hb * 4:(hb + 1) * 4, :], X1F[:, hb * 4:(hb + 1) * 4, :], X1F)
                else:
                    p.cp('act', x1t, x1t[:, hb * 4:(hb + 1) * 4, :], bk[:, :].rearrange("p (c t) -> p c t", c=4), bk)
            p.dma('sp', None, G['x1T_d'][:, :, t0:t0 + 128], x1t[:], x1t)
            if moe and RMODE >= 2:
                for c in range(8):
                    p.mm(b5, b5[:, 0:8], X1F[:, c, :], rt[:, c, :], [X1F, rt], start=(c == 0), stop=(c == 7))
                p.cp('dve', lgt, lgt[:], b5[:, 0:8], b5)
            if moe and RMODE >= 3:
                p.add('dve', lambda e: e.max(out=top8[:], in_=lgt[:]), r=[lgt], w=[top8])
                p.ts('dve', msk, msk[:], lgt[:], top8[:, 1:2], None, ALU.is_ge, None, [lgt, top8])
                p.ts('dve', nm1, nm1[:], top8[:, 0:1], -1.0, None, ALU.mult, None, top8)
                p.act(ex, ex[:], lgt[:], AF.Exp, [lgt, nm1], bias=nm1[:, 0:1])
                p.tt('dve', ex, ex[:], ex[:], msk[:], ALU.mult, [ex, msk])
                p.red(den, den[:], ex[:], ex)
                p.rcp(den, den[:], den[:], den)
                p.ts('dve', cmb, cmb[:], ex[:], den[:, 0:1], None, ALU.mult, None, [ex, den])
                p.dma('sp', None, G['comb_d'][t0:t0 + 128, :], cmb[:], cmb)
    p.es = G['es']
    p.barrier()


def phase5(p, C, G, l, xout):
    banks = G['banks']
    W = G['W']
    moe = (l % 2 == 1)
    li = l // 2
    HT = S // 2
    NTH = HT // 128
    if moe:
        experts = list(range(NE))
        fcs = [(k * 512, 512) for k in range(DFE // 512)]
        wg_ = lambda e: W['moe_w_gate'][li, e]
        wu_ = lambda e: W['moe_w_up'][li, e]
        wd_ = lambda e: W['moe_w_down'][li, e]
    else:
        experts = [0]
        fcs = [(k * 512, 512) for k in range(DFF // 512)] + [(DFF - DFF % 512, DFF % 512)]
        wg_ = lambda e: W['ffn_w_gate'][li]
        wu_ = lambda e: W['ffn_w_up'][li]
        wd_ = lambda e: W['ffn_w_down'][li]
    with ExitStack() as es:
        p.es = es
        g2 = p.sb([128, 1024], F32, 'g2')
        b2_ = p.sb([128, 1024], F32, 'b2_')
        p.dma('sp', g2, g2[:], W['ln2_g'][l:l + 1, :].partition_broadcast(128))
        p.dma('sp', b2_, b2_[:], W['ln2_b'][l:l + 1, :].partition_broadcast(128))
        wk = dict(st=p.sb([128, 12], F32, 'st'), mv=p.sb([128, 2], F32, 'mv'), rs=p.sb([128, 1], F32, 'rs'),
                  eps=p.sb([128, 1], F32, 'eps'))
        p.ms('pool', wk['eps'], wk['eps'][:], LN_EPS)
        xT = p.sb([128, 8, HT], BF16, 'xT')
        xTt = [T(xT.h, 'xT%d' % i) for i in range(HT // 512)]
        yacc = p.sb([128, NTH, 1024], F32, 'yacc')
        yt = [T(yacc.h, 'yacc%d' % i) for i in range(NTH)]
        comb = p.sb([128, NTH, 8], F32, 'comb')
        WG = [p.sb([128, 8, 512], BF16, 'WG') for _ in range(2)]
        WU = [p.sb([128, 8, 512], BF16, 'WU') for _ in range(2)]
        WD = [p.sb([128, 4, 1024], BF16, 'WD') for _ in range(2)]
        H1 = [p.sb([128, 4, 512], BF16, 'H1') for _ in range(2)]
        SG = [p.sb([128, 512], F32, 'SG') for _ in range(2)]
        OUT = [p.sb([128, 1024], F32, 'OUT') for _ in range(2)]
        units = [(e, fc) for e in experts for fc in fcs]

        def loadgu(ui):
            e, (f0, fs) = units[ui]
            b = ui % 2
            p.dma('pool', WG[b], WG[b][:, :, 0:fs], wg_(e)[:, f0:f0 + fs].rearrange("(c p) f -> p c f", p=128))
            p.dma('pool', WU[b], WU[b][:, :, 0:fs], wu_(e)[:, f0:f0 + fs].rearrange("(c p) f -> p c f", p=128))

        def loadd(ui):
            e, (f0, fs) = units[ui]
            b = ui % 2
            nft = fs // 128
            p.dma('pool', WD[b], WD[b][:, 0:nft, :], wd_(e)[f0:f0 + fs, :].rearrange("(c p) d -> p c d", p=128))

        state = dict(hi=0, gi=0, yi=0)

        def stage_a(ui, tg):
            e, (f0, fs) = units[ui]
            wg, wu = WG[ui % 2], WU[ui % 2]
            nft = fs // 128
            h1 = H1[state['hi'] % 2]
            state['hi'] += 1
            tsl = slice(tg * 512, (tg + 1) * 512)
            for ft in range(nft):
                gi = state['gi']
                pg = banks[(gi % 2) * 2]
                pu = banks[(gi % 2) * 2 + 1]
                sg = SG[gi % 2]
                state['gi'] += 1
                fsl = slice(ft * 128, (ft + 1) * 128)
                for c in range(8):
                    p.mm(pg, pg[:, :], wg[:, c, fsl], xT[:, c, tsl], [wg, xTt[tg]], start=(c == 0), stop=(c == 7))
                for c in range(8):
                    p.mm(pu, pu[:, :], wu[:, c, fsl], xT[:, c, tsl], [wu, xTt[tg]], start=(c == 0), stop=(c == 7))
                p.act(sg, sg[:], pg[:, :], AF.Silu, pg)
                p.tt('dve', h1, h1[:, ft, :], sg[:], pu[:, :], ALU.mult, [sg, pu])
            return (ui, tg, h1)

        def stage_b(ui, tg, h1):
            e, (f0, fs) = units[ui]
            wd = WD[ui % 2]
            nft = fs // 128
            for tt_ in range(4):
                ti = tg * 4 + tt_
                for dc in range(2):
                    py = banks[4 + (state['yi'] % 4)]
                    state['yi'] += 1
                    cs = slice(dc * 512, (dc + 1) * 512)
                    for ft in range(nft):
                        p.mm(py, py[:, :], h1[:, ft, tt_ * 128:(tt_ + 1) * 128], wd[:, ft, cs], [h1, wd],
                             start=(ft == 0), stop=(ft == nft - 1))
                    if moe:
                        p.stt(yt[ti], yacc[:, ti, cs], py[:, :], comb[:, ti, e:e + 1], yacc[:, ti, cs],
                              ALU.mult, ALU.add, [py, comb, yt[ti]])
                    else:
                        p.tt('dve', yt[ti], yacc[:, ti, cs], yacc[:, ti, cs], py[:, :], ALU.add, [yt[ti], py])

        NTG = HT // 512
        for half in range(2):
            tb = half * HT
            for tg_ in range(HT // 512):
                p.dma('sp', xTt[tg_], xT[:, :, tg_ * 512:(tg_ + 1) * 512],
                      G['x1T_d'][:, :, tb + tg_ * 512:tb + (tg_ + 1) * 512])
            for i in range(NTH):
                p.dma('sp', yt[i], yacc[:, i, :], G['x1_d'][tb + i * 128:tb + (i + 1) * 128, :])
                p.ts('dve' if i % 2 == 0 else 'pool', yt[i], yacc[:, i, :], yacc[:, i, :], ALPHA, None, ALU.mult, None, yt[i])
            if moe:
                p.dma('sp', comb, comb[:], G['comb_d'][tb:tb + HT, :].rearrange("(i p) e -> p i e", p=128))
            loadgu(0)
            loadd(0)
            pend = None
            for ui in range(len(units)):
                for tg in range(NTG):
                    cur = stage_a(ui, tg)
                    if pend is not None:
                        stage_b(*pend)
                    pend = cur
                    if tg == 0 and ui + 1 < len(units):
                        loadgu(ui + 1)
                        loadd(ui + 1)
            stage_b(*pend)
            for i in range(NTH):
                o = OUT[i % 2]
                st, mv, rs = wk['st'], wk['mv'], wk['rs']
                z = yacc[:, i, :]
                for hf in range(2):
                    p.add('dve', (lambda hf=hf, i=i: (lambda en: en.bn_stats(out=st[:, hf * 6:(hf + 1) * 6],
                                                                           in_=yacc[:, i, hf * 512:(hf + 1) * 512])))(),
                          r=[yt[i]], w=[st])
                p.add('dve', lambda en: en.bn_aggr(out=mv[:], in_=st[:]), r=[st], w=[mv])
                p.act(rs, rs[:], mv[:, 1:2], AF.Sqrt, [mv, wk['eps']], bias=wk['eps'][:, 0:1])
                p.rcp(rs, rs[:], rs[:], rs)
                p.ts('dve', yt[i], z, z, mv[:, 0:1], rs[:, 0:1], ALU.subtract, ALU.mult, [yt[i], mv, rs])
                p.tt('dve', yt[i], z, z, g2[:], ALU.mult, [yt[i], g2])
                p.tt('dve', o, o[:], z, b2_[:], ALU.add, [yt[i], b2_])
                p.dma('sp', None, xout[tb + i * 128:tb + (i + 1) * 128, :], o[:], o)
    p.es = G['es']
    p.barrier()


WNAMES = ['w_in', 'w_in_vres', 'shift_mu', 'shift_mu_vres', 'decay_up', 'decay_base', 'aaa_up', 'aaa_base',
          'vres_up', 'vres_base', 'gate_up', 'k_k', 'k_a', 'r_k', 'lnx_g', 'lnx_b', 'w_out', 'ln1_g', 'ln1_b',
          'ln2_g', 'ln2_b', 'ffn_w_gate', 'ffn_w_up', 'ffn_w_down', 'router', 'moe_w_gate', 'moe_w_up', 'moe_w_down']


def build_program(shapes, nlayers=DEPTH, stop_phase=None, dbg=(), layers=None):
    nc = bass.Bass("TRN2", target_bir_lowering=False)
    x = nc.dram_tensor("x", [S, D], F32, kind="ExternalInput").ap()
    W = {n: nc.dram_tensor(n, list(shapes[n]), F32, kind="ExternalInput").ap() for n in WNAMES}
    out = nc.dram_tensor("out", [S, D], F32, kind="ExternalOutput").ap()

    def scr(name, shape, dt):
        kind = "ExternalOutput" if name in dbg else "Internal"
        return nc.dram_tensor(name, shape, dt, kind=kind).ap()

    G = {'W': W}
    G['qk_d'] = scr('qk_d', [8, 128, S], BF16)
    G['att_d'] = scr('att_d', [512, S], BF16)
    G['g_d'] = scr('g_d', [S, 512], F32)
    G['v_d'] = scr('v_d', [S, 512], F32)
    G['vfirst_d'] = scr('vfirst_d', [S, 512], F32)
    G['rk_d'] = scr('rk_d', [S, 8], F32)
    G['y_d'] = scr('y_d', [S, 512], F32)
    G['gc_d'] = scr('gc_d', [S // 64, 64, 8], F32)
    G['tm_d'] = scr('tm_d', [S // 64, 64, 4, 512], BF16)
    G['ft_d'] = scr('ft_d', [S // 64, 64, 2048], BF16)
    G['x1_d'] = scr('x1_d', [S, D], F32)
    G['x1T_d'] = scr('x1T_d', [128, 8, S], BF16)
    G['comb_d'] = scr('comb_d', [S, 8], F32)
    G['xcur_d'] = scr('xcur_d', [S, D], F32)
    with ExitStack() as es:
        p = Prog(nc, es)
        G['es'] = es
        C = build_consts(p)
        G['banks'] = [p.ps([128, 512], F32, 'bank%d' % i) for i in range(8)]
        p.barrier()
        done = False
        lay = list(range(nlayers)) if layers is None else list(layers)
        for l in lay:
            xin = x if l == lay[0] else G['xcur_d']
            xout = out if (l == lay[-1] and stop_phase is None) else G['xcur_d']
            phases = [lambda: phase1a(p, C, G, l, xin), lambda: phase1b(p, C, G, l), lambda: phase2(p, C, G, l),
                      lambda: phase3(p, C, G, l), lambda: phase4(p, C, G, l, xin), lambda: phase5(p, C, G, l, xout)]
            with ExitStack() as esf:
                p.es = esf
                G['es'] = esf
                hT = p.sb([128, 8, S + 2], BF16, 'hT')
                G['hT'] = hT
                G['hTt'] = [T(hT.h, 'hT%d' % i) for i in range(NT)]
                p.ms('pool', G['hTt'][0], hT[:, :, 0:2], 0.0)
                for pi in range(3):
                    phases[pi]()
                    if stop_phase is not None and (l, pi) == tuple(stop_phase):
                        done = True
                        break
            p.es = es
            G['es'] = es
            if not done:
                for pi in range(3, 6):
                    phases[pi]()
                    if stop_phase is not None and (l, pi) == tuple(stop_phase):
                        done = True
                        break
            if done:
                break
        if done:
            with ExitStack() as es2:
                p.es = es2
                tmp = p.sb([128, 1024], F32, 'dbgtmp')
                p.dma('sp', tmp, tmp[:], x[0:128, :])
                p.dma('sp', None, out[0:128, :], tmp[:], tmp)
            p.es = es
            p.barrier()
        stats = p.emit()
    return nc, stats


_CACHE = {}


def kernel(**inputs):
    x = np.ascontiguousarray(inputs['x'], dtype=np.float32)
    B = x.shape[0]
    shapes = {n: inputs[n].shape for n in WNAMES}
    if 'nc' not in _CACHE:
        _CACHE['nc'] = build_program(shapes)[0]
    nc = _CACHE['nc']
    wmap = {n: np.ascontiguousarray(inputs[n], dtype=np.float32) for n in WNAMES}
    in_maps = []
    for b in range(B):
        m = dict(wmap)
        m['x'] = x[b]
        in_maps.append(m)
    res = run_bass_kernel_spmd(nc, in_maps, core_ids=list(range(B)))
    return np.stack([np.asarray(r['out'], dtype=np.float32).reshape(S, D) for r in res.results], axis=0)
```
